# Optimizing a Trainium2 kernel written in Bass

```python
import jax, jax.numpy as jnp
from jax import lax
import numpy as np

D_MODEL = 2048
BATCH = 4
SEQ = 8192
DEPTH = 1

HEAD_DIM = 128
SWA_GROUPS = ((128, 1), (512, 4), (2048, 16))
SWA_HEADS_PER_GROUP = 4
SWA_HEADS = SWA_HEADS_PER_GROUP * len(SWA_GROUPS)
SWA_WIDTH = SWA_HEADS * HEAD_DIM
SWA_OUT_WIDTH = SWA_HEADS_PER_GROUP * HEAD_DIM
SWA_BLOCK = 64
ROPE_THETA = 500000.0
ROPE_DIMS = HEAD_DIM // 4
GDN_HEADS = 12
GDN_WIDTH = GDN_HEADS * HEAD_DIM
GDN_CONV = 5
GDN_CHUNK = 64
MEM_TOKENS = 256
MEM_HEADS = 4
MEM_HEAD_DIM = 256
MEM_WIDTH = MEM_HEADS * MEM_HEAD_DIM
N_BRANCH = 3
IN_SPLITS = (SWA_WIDTH, SWA_WIDTH, SWA_WIDTH,
             GDN_WIDTH, GDN_WIDTH, GDN_WIDTH, GDN_WIDTH,
             2 * GDN_HEADS, 2 * GDN_HEADS,
             MEM_WIDTH, N_BRANCH * D_MODEL)
IN_WIDTH = sum(IN_SPLITS)
N_GROUPS = 4
EXPERTS_PER_GROUP = 8
N_EXPERTS = N_GROUPS * EXPERTS_PER_GROUP
EXPERT_TOPK = 2
D_EXPERT = 512
MOE_BLOCK = 256
EPS = 1e-6
NEG_INF = -1e30

kernel_name = "hybrid_gated_dilated_swa_gdn_memxattn_hiermoe"


def rmsnorm(x, g):
    xf = x.astype(jnp.float32)
    r = lax.rsqrt(jnp.mean(xf * xf, axis=-1, keepdims=True) + EPS)
    return (xf * r).astype(x.dtype) * g


def l2norm(t):
    tf = t.astype(jnp.float32)
    return (tf * lax.rsqrt(jnp.sum(tf * tf, axis=-1, keepdims=True) + EPS)).astype(t.dtype)


def partial_rope(x, pos):
    half = ROPE_DIMS // 2
    inv = ROPE_THETA ** (-jnp.arange(half, dtype=jnp.float32) / half)
    ang = pos.astype(jnp.float32)[:, None] * inv[None, :]
    cos = jnp.cos(ang)[None, :, None, :]
    sin = jnp.sin(ang)[None, :, None, :]
    x1 = x[..., :half].astype(jnp.float32)
    x2 = x[..., half:ROPE_DIMS].astype(jnp.float32)
    rot = jnp.concatenate([x1 * cos - x2 * sin, x2 * cos + x1 * sin], axis=-1).astype(x.dtype)
    return jnp.concatenate([rot, x[..., ROPE_DIMS:]], axis=-1)


def banded_attention(q, k, v, radius):
    N, L, H, hd = q.shape
    blk = SWA_BLOCK
    nb = -(-L // blk)
    Lp = nb * blk
    pad = Lp - L
    qb = jnp.pad(q, ((0, 0), (0, pad), (0, 0), (0, 0))).reshape(N, nb, blk, H, hd)
    kp = jnp.pad(k, ((0, 0), (blk, pad + blk), (0, 0), (0, 0))).reshape(N, nb + 2, blk, H, hd)
    vp = jnp.pad(v, ((0, 0), (blk, pad + blk), (0, 0), (0, 0))).reshape(N, nb + 2, blk, H, hd)
    kb = jnp.concatenate([kp[:, :-2], kp[:, 1:-1], kp[:, 2:]], axis=2)
    vb = jnp.concatenate([vp[:, :-2], vp[:, 1:-1], vp[:, 2:]], axis=2)
    s = jnp.einsum('ncqhd,nckhd->nchqk', qb, kb,
                   preferred_element_type=jnp.float32) * (hd ** -0.5)
    qpos = jnp.arange(nb)[:, None] * blk + jnp.arange(blk)[None, :]
    kpos = (jnp.arange(nb)[:, None] - 1) * blk + jnp.arange(3 * blk)[None, :]
    off = kpos[:, None, :] - qpos[:, :, None]
    valid = (jnp.abs(off) <= radius) & (kpos[:, None, :] >= 0) & (kpos[:, None, :] < L)
    s = jnp.where(valid[None, :, None], s, NEG_INF)
    m = jnp.max(s, axis=-1, keepdims=True)
    p = jnp.exp(s - m)
    l = jnp.sum(p, axis=-1, keepdims=True)
    o = jnp.einsum('nchqk,nckhd->ncqhd', (p / l).astype(v.dtype), vb)
    lse = (m + jnp.log(l))[..., 0].transpose(0, 1, 3, 2).reshape(N, Lp, H)
    return o.reshape(N, Lp, H, hd)[:, :L], lse[:, :L]


def dilated_swa(q, k, v):
    B, S, _, hd = q.shape
    H = SWA_HEADS_PER_GROUP
    outs, lses = [], []
    for g, (window, dil) in enumerate(SWA_GROUPS):
        radius = window // (2 * dil)
        Ld = S // dil

        def to_residues(t):
            t = t[:, :, g * H:(g + 1) * H]
            return t.reshape(B, Ld, dil, H, hd).transpose(0, 2, 1, 3, 4).reshape(B * dil, Ld, H, hd)

        o, lse = banded_attention(to_residues(q), to_residues(k), to_residues(v), radius)
        outs.append(o.reshape(B, dil, Ld, H, hd).transpose(0, 2, 1, 3, 4).reshape(B, S, H, hd))
        lses.append(lse.reshape(B, dil, Ld, H).transpose(0, 2, 1, 3).reshape(B, S, H))
    alpha = jax.nn.softmax(jnp.stack(lses), axis=0)
    return jnp.einsum('gbsh,gbshd->bshd', alpha.astype(q.dtype), jnp.stack(outs))


def centred_depthwise_conv(x, w):
    K, C = w.shape
    return lax.conv_general_dilated(
        x, w[:, None, :].astype(x.dtype), window_strides=(1,),
        padding=[(K // 2, K // 2)], dimension_numbers=('NWC', 'WIO', 'NWC'),
        feature_group_count=C)


def gdn_chunked(q, k, v, g, beta):
    f32 = jnp.float32
    lead = q.shape[:-2]
    S, dk = q.shape[-2:]
    dv = v.shape[-1]
    C = GDN_CHUNK
    nc = S // C
    q = q.astype(f32).reshape(*lead, nc, C, dk)
    k = k.astype(f32).reshape(*lead, nc, C, dk)
    v = v.astype(f32).reshape(*lead, nc, C, dv)
    b = beta.astype(f32).reshape(*lead, nc, C, 1)
    gc = jnp.cumsum(g.astype(f32).reshape(*lead, nc, C), axis=-1)
    i = jnp.arange(C)
    strict = i[:, None] > i[None, :]
    incl = i[:, None] >= i[None, :]
    decay = jnp.exp(jnp.where(incl, gc[..., :, None] - gc[..., None, :], -jnp.inf))
    kb = k * b
    lmat = jnp.where(strict, jnp.einsum('...id,...jd->...ij', kb, k) * decay, 0.0)
    egc = jnp.exp(gc)[..., None]
    u = lax.linalg.triangular_solve(lmat, jnp.concatenate([v * b, kb * egc], axis=-1),
                                    left_side=True, lower=True, unit_diagonal=True)
    value, kcd = u[..., :dv], u[..., dv:]
    attn = jnp.einsum('...id,...jd->...ij', q, k) * decay
    q_e = q * egc
    k_e = k * jnp.exp(gc[..., -1:] - gc)[..., None]
    g_e = jnp.exp(gc[..., -1])[..., None, None]
    xs = tuple(jnp.moveaxis(t, -3, 0) for t in (value, kcd, attn, q_e, k_e, g_e))

    def step(state, c):
        value_c, kcd_c, attn_c, qe_c, ke_c, ge_c = c
        v_new = value_c - jnp.einsum('...ck,...kv->...cv', kcd_c, state)
        o = (jnp.einsum('...ck,...kv->...cv', qe_c, state)
             + jnp.einsum('...ij,...jv->...iv', attn_c, v_new))
        state = state * ge_c + jnp.einsum('...ck,...cv->...kv', ke_c, v_new)
        return state, o

    state0 = jnp.zeros((*lead, dk, dv), f32)
    _, o = lax.scan(step, state0, xs)
    return jnp.moveaxis(o, 0, -3).reshape(*lead, S, dv)


def bidirectional_gdn(q, k, v, g, beta):
    def dirs(t):
        return jnp.stack([t, t[:, ::-1]]).transpose(0, 1, 3, 2, 4)

    gd = jnp.stack([g[:, :, 0], g[:, ::-1, 1]]).transpose(0, 1, 3, 2)
    bd = jnp.stack([beta[:, :, 0], beta[:, ::-1, 1]]).transpose(0, 1, 3, 2)
    o = gdn_chunked(dirs(q), dirs(k), dirs(v), gd, bd)
    o = o[0] + o[1][:, :, ::-1]
    return o.transpose(0, 2, 1, 3).astype(q.dtype)


def hier_moe(x, w_route_group, b_route_group, w_route_expert, w_expert_gate, w_expert_up, w_expert_down):
    B, S, D = x.shape
    T = B * S
    K = EXPERT_TOPK
    xt = x.reshape(T, D)
    grp_logits = jnp.dot(xt, w_route_group, preferred_element_type=jnp.float32) + b_route_group.astype(jnp.float32)
    grp = jnp.argmax(grp_logits, axis=-1)
    p_grp = jnp.take_along_axis(jax.nn.softmax(grp_logits, axis=-1), grp[:, None], axis=-1)
    exp_logits = jnp.dot(xt, w_route_expert, preferred_element_type=jnp.float32).reshape(T, N_GROUPS, EXPERTS_PER_GROUP)
    sel = jnp.take_along_axis(exp_logits, grp[:, None, None], axis=1)[:, 0]
    top_val, top_idx = lax.top_k(sel, K)
    wts = jax.nn.softmax(top_val, axis=-1) * p_grp
    eid = (grp[:, None] * EXPERTS_PER_GROUP + top_idx).astype(jnp.int32)
    N = T * K
    e_flat = eid.reshape(N)
    t_flat = jnp.repeat(jnp.arange(T, dtype=jnp.int32), K)
    w_flat = wts.reshape(N)
    order = jnp.argsort(e_flat)
    e_s, t_s, w_s = e_flat[order], t_flat[order], w_flat[order]
    counts = jnp.bincount(e_flat, length=N_EXPERTS)
    padded = (counts + MOE_BLOCK - 1) // MOE_BLOCK * MOE_BLOCK
    start = jnp.cumsum(counts) - counts
    pad_end = jnp.cumsum(padded)
    pad_start = pad_end - padded
    dest = pad_start[e_s] + (jnp.arange(N, dtype=jnp.int32) - start[e_s])
    nblk = -(-N // MOE_BLOCK) + N_EXPERTS
    P = nblk * MOE_BLOCK
    slot_tok = jnp.zeros((P,), jnp.int32).at[dest].set(t_s)
    slot_w = jnp.zeros((P,), jnp.float32).at[dest].set(w_s)
    blk_expert = jnp.minimum(jnp.searchsorted(pad_end, jnp.arange(nblk) * MOE_BLOCK, side='right'),
                             N_EXPERTS - 1).astype(jnp.int32)

    def expert_block(args):
        tok, e = args
        xb = xt[tok]
        hmid = jax.nn.silu(xb @ w_expert_gate[e]) * (xb @ w_expert_up[e])
        return hmid @ w_expert_down[e]

    yb = lax.map(expert_block, (slot_tok.reshape(nblk, MOE_BLOCK), blk_expert))
    y = yb.reshape(P, D) * slot_w[:, None].astype(x.dtype)
    return jnp.zeros((T, D), x.dtype).at[slot_tok].add(y).reshape(B, S, D)


def hybrid_layer(x, mem, g_mix, w_in, b_gate, gdn_conv, gdn_a_log, gdn_dt_bias, gdn_norm_g,
                 g_mem, w_mem_kv, w_o_swa, w_o_gdn, w_o_mem, w_out, g_ffn,
                 w_route_group, b_route_group, w_route_expert, w_expert_gate, w_expert_up, w_expert_down):
    B, S, D = x.shape
    hd = HEAD_DIM
    a = rmsnorm(x, g_mix)
    proj = a @ w_in
    (aq, ak, av, bq, bk, bv, bz, b_beta, b_alpha, mq, gate_logits) = jnp.split(
        proj, [int(i) for i in np.cumsum(IN_SPLITS)[:-1]], axis=-1)
    pos = jnp.arange(S)

    aq = partial_rope(aq.reshape(B, S, SWA_HEADS, hd), pos)
    ak = partial_rope(ak.reshape(B, S, SWA_HEADS, hd), pos)
    o_a = dilated_swa(aq, ak, av.reshape(B, S, SWA_HEADS, hd))
    y_a = o_a.reshape(B, S, SWA_OUT_WIDTH) @ w_o_swa

    qkv = jax.nn.silu(centred_depthwise_conv(jnp.concatenate([bq, bk, bv], axis=-1), gdn_conv))
    q_b, k_b, v_b = jnp.split(qkv, [GDN_WIDTH, 2 * GDN_WIDTH], axis=-1)
    q_b = l2norm(q_b.reshape(B, S, GDN_HEADS, hd)) * (hd ** -0.5)
    k_b = l2norm(k_b.reshape(B, S, GDN_HEADS, hd))
    v_b = v_b.reshape(B, S, GDN_HEADS, hd)
    beta = jax.nn.sigmoid(b_beta.reshape(B, S, 2, GDN_HEADS).astype(jnp.float32))
    g_log = -jnp.exp(gdn_a_log.astype(jnp.float32)) * jax.nn.softplus(
        b_alpha.reshape(B, S, 2, GDN_HEADS).astype(jnp.float32) + gdn_dt_bias.astype(jnp.float32))
    o_b = bidirectional_gdn(q_b, k_b, v_b, g_log, beta)
    o_b = rmsnorm(o_b, gdn_norm_g) * jax.nn.silu(bz.reshape(B, S, GDN_HEADS, hd))
    y_b = o_b.reshape(B, S, GDN_WIDTH) @ w_o_gdn

    kv = rmsnorm(mem, g_mem) @ w_mem_kv
    mk, mv = jnp.split(kv, 2, axis=-1)
    mk = mk.reshape(B, MEM_TOKENS, MEM_HEADS, MEM_HEAD_DIM)
    mv = mv.reshape(B, MEM_TOKENS, MEM_HEADS, MEM_HEAD_DIM)
    s_m = jnp.einsum('bshd,bmhd->bhsm', mq.reshape(B, S, MEM_HEADS, MEM_HEAD_DIM), mk,
                     preferred_element_type=jnp.float32) * (MEM_HEAD_DIM ** -0.5)
    p_m = jax.nn.softmax(s_m, axis=-1).astype(mv.dtype)
    o_m = jnp.einsum('bhsm,bmhd->bshd', p_m, mv)
    y_m = o_m.reshape(B, S, MEM_WIDTH) @ w_o_mem

    gates = jax.nn.sigmoid(gate_logits.reshape(B, S, N_BRANCH, D) + b_gate)
    mix = gates[:, :, 0] * y_a + gates[:, :, 1] * y_b + gates[:, :, 2] * y_m
    x = x + mix @ w_out

    x = x + hier_moe(rmsnorm(x, g_ffn), w_route_group, b_route_group, w_route_expert,
                     w_expert_gate, w_expert_up, w_expert_down)
    return x


def setup_inputs(seed: int = 0) -> dict:
    key = jax.random.key(seed)
    ks = jax.random.split(key, 24)
    f32 = jnp.float32
    L = DEPTH

    def nrm(k, shape, fan):
        return jax.random.normal(k, shape, f32) * (fan ** -0.5)

    def gain(k, shape):
        return 1.0 + 0.02 * jax.random.normal(k, shape, f32)

    dt = jax.random.uniform(ks[7], (L, 2, GDN_HEADS), f32, 1e-3, 1e-1)
    return {
        "x": jax.random.normal(ks[0], (BATCH, SEQ, D_MODEL), f32),
        "mem": jax.random.normal(ks[1], (BATCH, MEM_TOKENS, D_MODEL), f32),
        "g_mix": gain(ks[2], (L, D_MODEL)),
        "w_in": nrm(ks[3], (L, D_MODEL, IN_WIDTH), D_MODEL),
        "b_gate": 0.02 * jax.random.normal(ks[4], (L, N_BRANCH, D_MODEL), f32),
        "gdn_conv": nrm(ks[5], (L, GDN_CONV, 3 * GDN_WIDTH), GDN_CONV),
        "gdn_a_log": jnp.log(jax.random.uniform(ks[6], (L, 2, GDN_HEADS), f32, 1.0, 16.0)),
        "gdn_dt_bias": dt + jnp.log(-jnp.expm1(-dt)),
        "gdn_norm_g": gain(ks[8], (L, HEAD_DIM)),
        "g_mem": gain(ks[9], (L, D_MODEL)),
        "w_mem_kv": nrm(ks[10], (L, D_MODEL, 2 * MEM_WIDTH), D_MODEL),
        "w_o_swa": nrm(ks[11], (L, SWA_OUT_WIDTH, D_MODEL), SWA_OUT_WIDTH),
        "w_o_gdn": nrm(ks[12], (L, GDN_WIDTH, D_MODEL), GDN_WIDTH),
        "w_o_mem": nrm(ks[13], (L, MEM_WIDTH, D_MODEL), MEM_WIDTH),
        "w_out": nrm(ks[14], (L, D_MODEL, D_MODEL), D_MODEL),
        "g_ffn": gain(ks[15], (L, D_MODEL)),
        "w_route_group": nrm(ks[16], (L, D_MODEL, N_GROUPS), D_MODEL),
        "b_route_group": 0.01 * jax.random.normal(ks[17], (L, N_GROUPS), f32),
        "w_route_expert": nrm(ks[18], (L, D_MODEL, N_EXPERTS), D_MODEL),
        "w_expert_gate": nrm(ks[19], (L, N_EXPERTS, D_MODEL, D_EXPERT), D_MODEL),
        "w_expert_up": nrm(ks[20], (L, N_EXPERTS, D_MODEL, D_EXPERT), D_MODEL),
        "w_expert_down": nrm(ks[21], (L, N_EXPERTS, D_EXPERT, D_MODEL), D_EXPERT),
        "g_final": gain(ks[22], (D_MODEL,)),
    }


def reference(x, mem, g_mix, w_in, b_gate, gdn_conv, gdn_a_log, gdn_dt_bias, gdn_norm_g,
              g_mem, w_mem_kv, w_o_swa, w_o_gdn, w_o_mem, w_out, g_ffn,
              w_route_group, b_route_group, w_route_expert, w_expert_gate, w_expert_up,
              w_expert_down, g_final):
    h = x
    for layer in range(DEPTH):
        h = hybrid_layer(h, mem, g_mix[layer], w_in[layer], b_gate[layer], gdn_conv[layer],
                         gdn_a_log[layer], gdn_dt_bias[layer], gdn_norm_g[layer], g_mem[layer],
                         w_mem_kv[layer], w_o_swa[layer], w_o_gdn[layer], w_o_mem[layer], w_out[layer],
                         g_ffn[layer], w_route_group[layer], b_route_group[layer], w_route_expert[layer],
                         w_expert_gate[layer], w_expert_up[layer], w_expert_down[layer])
    return rmsnorm(h, g_final)
```

```python
from contextlib import ExitStack
import numpy as np
import concourse.bass as bass
import concourse.mybir as mybir
from concourse.bass_utils import run_bass_kernel_spmd

F32 = mybir.dt.float32
BF16 = mybir.dt.bfloat16
AF = mybir.ActivationFunctionType
ALU = mybir.AluOpType

D_MODEL = 2048
SEQ = 8192
NLOC = 8192
NOWN = 4096
NSWA = 5120
HD = 128
IN_WIDTH = 17968
C_AQ, C_AK, C_AV, C_BQ, C_BK, C_BV, C_BZ, C_BETA, C_ALPHA, C_MQ, C_GATE = (
    0, 1536, 3072, 4608, 6144, 7680, 9216, 10752, 10776, 10800, 11824)
EPS = 1e-6
NEG = -1e30
POOL_ARITH = False


class V:
    __slots__ = ("buf", "ap")

    def __init__(self, buf, ap):
        self.buf = buf
        self.ap = ap

    def __getitem__(self, idx):
        return V(self.buf, self.ap[idx])

    def re(self, pat, **kw):
        return V(self.buf, self.ap.rearrange(pat, **kw))


class Buf:
    __slots__ = ("t", "w", "r", "name")

    def __init__(self, t, name=""):
        self.t = t
        self.w = None
        self.r = {}
        self.name = name

    def __getitem__(self, idx):
        return V(self, self.t[idx])

    def re(self, pat, **kw):
        return V(self, self.t[:].rearrange(pat, **kw))


class Sched:
    CAP = 30000

    def __init__(self, nc, ndma=48):
        self.nc = nc
        self.E = {"pe": nc.tensor, "act": nc.scalar, "dve": nc.vector, "pool": nc.gpsimd, "sp": nc.sync}
        self.cnt = {e: 0 for e in self.E}
        self.sems = {e: [] for e in self.E}
        self.seen = {e: {} for e in self.E}
        self.dsem = [nc.alloc_semaphore(f"dq{i}") for i in range(ndma)]
        self.dcnt = [0] * ndma
        self.dpool = {"sp": list(range(0, ndma - 16)), "pool": list(range(ndma - 16, ndma - 4)),
                      "act": list(range(ndma - 4, ndma))}
        self.drr = {q: 0 for q in self.dpool}
        self.nwait = 0
        self.ninst = 0

    def _sem(self, e, k):
        while len(self.sems[e]) <= k:
            self.sems[e].append(self.nc.alloc_semaphore(f"s_{e}_{len(self.sems[e])}"))
        return self.sems[e][k]

    def _wait(self, e, tok):
        if tok is None:
            return
        kind, a, c = tok
        if kind == "e":
            if a == e and e == "pe":
                return
            key = ("e", a)
            if self.seen[e].get(key, 0) >= c:
                return
            self.seen[e][key] = c
            k, v = (c - 1) // self.CAP, (c - 1) % self.CAP + 1
            self.E[e].wait_ge(self._sem(a, k), v)
        else:
            key = ("d", a)
            if self.seen[e].get(key, 0) >= c:
                return
            self.seen[e][key] = c
            self.E[e].wait_ge(self.dsem[a], 16 * c)
        self.nwait += 1

    def _deps(self, e, reads, writes):
        for b in reads:
            self._wait(e, b.w)
        for b in writes:
            self._wait(e, b.w)
            for t in list(b.r.values()):
                self._wait(e, t)

    def _commit(self, tok, key, reads, writes):
        for b in reads:
            b.r[key] = tok
        for b in writes:
            b.w = tok
            b.r = {}

    def op(self, e, fn, reads=(), writes=()):
        self._deps(e, reads, writes)
        inst = fn(self.E[e])
        self.cnt[e] += 1
        c = self.cnt[e]
        inst.then_inc(self._sem(e, (c - 1) // self.CAP), 1)
        self.ninst += 1
        tok = ("e", e, c)
        self._commit(tok, ("e", e), reads, writes)
        return tok

    def dma(self, q, out, in_, **kw):
        reads, writes = [in_.buf], [out.buf]
        self._deps(q, reads, writes)
        pl = self.dpool[q]
        i = pl[self.drr[q] % len(pl)]
        self.drr[q] += 1
        if self.dcnt[i] > 0:
            self._wait(q, ("d", i, self.dcnt[i]))
        inst = self.E[q].dma_start(out=out.ap, in_=in_.ap, **kw)
        self.dcnt[i] += 1
        assert self.dcnt[i] * 16 < 60000
        inst.then_inc(self.dsem[i], 16)
        self.ninst += 1
        tok = ("d", i, self.dcnt[i])
        self._commit(tok, ("d", i), reads, writes)
        return tok

    def barrier(self):
        for e in self.E:
            for a in self.E:
                if self.cnt[a]:
                    self._wait(e, ("e", a, self.cnt[a]))
            for i, c in enumerate(self.dcnt):
                if c:
                    self._wait(e, ("d", i, c))

    def finish(self):
        for i, c in enumerate(self.dcnt):
            if c:
                self._wait("sp", ("d", i, c))
        for e in self.E:
            if e != "sp" and self.cnt[e]:
                self._wait("sp", ("e", e, self.cnt[e]))


class KB:
    def __init__(self, debug=()):
        self.nc = bass.Bass("TRN2", target_bir_lowering=False)
        self.S = Sched(self.nc)
        self.debug = set(debug)
        self.outs = []

    def dram_in(self, name, shape, dtype=F32):
        return Buf(self.nc.dram_tensor(name, list(shape), dtype, kind="ExternalInput").ap(), name)

    def dram_out(self, name, shape, dtype=F32):
        self.outs.append(name)
        return Buf(self.nc.dram_tensor(name, list(shape), dtype, kind="ExternalOutput").ap(), name)

    def dram_tmp(self, name, shape, dtype):
        if name in self.debug:
            return self.dram_out(name, shape, dtype)
        return Buf(self.nc.dram_tensor(name, list(shape), dtype, kind="Internal").ap(), name)

    def sb(self, es, name, shape, dtype=F32):
        self.uid = getattr(self, "uid", 0) + 1
        name = f"{name}_u{self.uid}"
        return Buf(es.enter_context(self.nc.sbuf_tensor(name, list(shape), dtype)), name)

    def ps(self, es, name, shape, dtype=F32):
        self.uid = getattr(self, "uid", 0) + 1
        name = f"{name}_u{self.uid}"
        return Buf(es.enter_context(self.nc.psum_tensor(name, list(shape), dtype)), name)

    def X(self, e, meth, **kw):
        reads, writes, args = [], [], {}
        for k, v in kw.items():
            if isinstance(v, V):
                (writes if k in ("out", "accum_out") else reads).append(v.buf)
                args[k] = v.ap
            else:
                args[k] = v
        return self.S.op(e, lambda eng: getattr(eng, meth)(**args), reads, writes)

    def MM(self, out, lhsT, rhs, start=True, stop=True):
        return self.S.op("pe", lambda eng: eng.matmul(out.ap, lhsT=lhsT.ap, rhs=rhs.ap, start=start, stop=stop),
                         [lhsT.buf, rhs.buf], [out.buf])

    def TR(self, out, in_, ident):
        return self.S.op("pe", lambda eng: eng.transpose(out.ap, in_.ap, ident.ap), [in_.buf, ident.buf], [out.buf])

    def D(self, q, out, in_, slow=False):
        if slow:
            return self.S.dma(q, out, in_, allow_slow_non_contiguous=True)
        return self.S.dma(q, out, in_)

    def act(self, out, in_, func, **kw):
        return self.X("act", "activation", out=out, in_=in_, func=func, **kw)

    def tt(self, e, out, in0, in1, op):
        if e == "pool" and not POOL_ARITH:
            e = "dve"
        return self.X(e, "tensor_tensor", out=out, in0=in0, in1=in1, op=op)

    def ts(self, e, out, in0, scalar1, op0, scalar2=None, op1=None):
        if e == "pool" and not POOL_ARITH:
            e = "dve"
        kw = dict(out=out, in0=in0, scalar1=scalar1, scalar2=scalar2, op0=op0)
        if op1 is not None:
            kw["op1"] = op1
        return self.X(e, "tensor_scalar", **kw)

    def stt(self, out, in0, scalar, op0, in1, op1):
        return self.X("dve", "scalar_tensor_tensor", out=out, in0=in0, scalar=scalar, op0=op0, in1=in1, op1=op1)

    def memset(self, e, out, val):
        return self.S.op(e, lambda eng: eng.memset(out.ap, val), [], [out.buf])

    def cp(self, e, out, in_):
        if e == "act":
            return self.act(out, in_, AF.Copy)
        return self.X(e, "tensor_copy", out=out, in_=in_)


class NS:
    pass


def tensor_specs():
    sp = {}

    def di(name, shape):
        sp[name] = ("in", shape, F32)

    def dt(name, shape, dtype):
        sp[name] = ("tmp", shape, dtype)
    di("xT", [D_MODEL, NLOC])
    di("memT", [D_MODEL, 256])
    di("w_in", [D_MODEL, IN_WIDTH])
    di("g_mix_c", [128, 16])
    di("g_mem_c", [128, 16])
    di("g_ffn_c", [128, 16])
    di("b_gate_c", [128, 48])
    di("convw", [128, 36, 5])
    di("alog_rep", [128, 64, 24])
    di("dtb_rep", [128, 64, 24])
    di("gnorm_rep", [128, 128])
    di("w_mem_kv", [D_MODEL, 2048])
    di("w_o_cat", [3072, D_MODEL])
    di("w_out", [D_MODEL, D_MODEL])
    di("w_route", [D_MODEL, 36])
    di("b_route_rep", [128, 4])
    di("w_eg", [32, D_MODEL, 512])
    di("w_eu", [32, D_MODEL, 512])
    di("w_ed", [32, 512, D_MODEL])
    di("g_final_rep", [128, D_MODEL])
    for nm in ("ident", "ones", "triA", "triB", "mSA", "mSB", "mTA", "mTB", "perm"):
        di("c_" + nm, [128, 128])
    di("c_ropeC", [128, NSWA])
    di("c_ropeS", [128, NSWA])
    di("c_band", [128, 2, 256])
    dt("w_in_b", [D_MODEL, IN_WIDTH], BF16)
    dt("w_mem_kv_b", [D_MODEL, 2048], BF16)
    dt("w_o_cat_b", [3072, D_MODEL], BF16)
    dt("w_out_b", [D_MODEL, D_MODEL], BF16)
    dt("w_eg_b", [32 * D_MODEL, 512], BF16)
    dt("w_eu_b", [32 * D_MODEL, 512], BF16)
    dt("w_ed_b", [32 * 512, D_MODEL], BF16)
    dt("aT_d", [D_MODEL, NLOC], BF16)
    dt("qa_d", [12, 128, NOWN], BF16)
    dt("ka_d", [12, 128, NSWA], BF16)
    dt("va_d", [NSWA, 1536], BF16)
    dt("pre_d", [36, 128, NLOC], BF16)
    dt("z_d", [NOWN, 1536], BF16)
    dt("ba_d", [NLOC, 48], F32)
    dt("mq_d", [8, 128, NOWN], BF16)
    dt("gate_d", [48, 128, NOWN], BF16)
    dt("oa_d", [3, NOWN, 512], BF16)
    dt("lse_d", [3, 4, NOWN, 1], F32)
    dt("oT_d", [24, 128, NOWN], BF16)
    dt("gq_d", [12, 128, NOWN], BF16)
    dt("gk_d", [12, 128, NLOC], BF16)
    dt("gkt_d", [12, NLOC, 128], BF16)
    dt("gv_d", [12, NLOC, 128], BF16)
    dt("ob_d", [2, NOWN, 1536], F32)
    dt("bg_d", [NLOC, 72], F32)
    dt("x1T_d", [16, 128, NOWN], F32)
    dt("xnT_d", [16, 128, NOWN], BF16)
    dt("wgt_d", [NOWN, 32], F32)
    sp["out"] = ("out", [NOWN, D_MODEL], F32)
    return sp


class Tens:
    def __init__(self, K):
        object.__setattr__(self, "_K", K)
        object.__setattr__(self, "_sp", tensor_specs())
        object.__setattr__(self, "in_names", [])

    def __getattr__(self, name):
        kind, shape, dtype = self._sp[name]
        K = self._K
        if kind == "in":
            b = K.dram_in(name, shape, dtype)
            self.in_names.append(name)
        elif kind == "out":
            b = K.dram_out(name, shape, dtype)
        else:
            b = K.dram_tmp(name, shape, dtype)
        object.__setattr__(self, name, b)
        return b


def load_consts(K, es, T):
    Cn = NS()
    for nm in ("ident", "ones", "triA", "triB", "mSA", "mSB", "mTA", "mTB"):
        b = K.sb(es, "k_" + nm, [128, 128], F32)
        K.D("sp", b[:], getattr(T, "c_" + nm)[:, :])
        setattr(Cn, nm, b)
    Cn.ident_b = K.sb(es, "k_ident_b", [128, 128], BF16)
    K.cp("dve", Cn.ident_b[:], Cn.ident[:])
    Cn.ones_b = K.sb(es, "k_ones_b", [128, 128], BF16)
    K.cp("dve", Cn.ones_b[:], Cn.ones[:])
    permf = K.sb(es, "k_perm_f", [128, 128], F32)
    K.D("sp", permf[:], T.c_perm[:, :])
    Cn.perm_b = K.sb(es, "k_perm_b", [128, 128], BF16)
    K.cp("dve", Cn.perm_b[:], permf[:])
    Cn.eps = K.sb(es, "k_eps", [128, 2], F32)
    K.memset("dve", Cn.eps[:], EPS)
    Cn.onec = K.sb(es, "k_onec", [128, 2], F32)
    K.memset("dve", Cn.onec[:], 1.0)
    for nm, w in (("g_mix_c", 16), ("g_mem_c", 16), ("g_ffn_c", 16), ("b_gate_c", 48)):
        b = K.sb(es, "k_" + nm, [128, w], F32)
        K.D("sp", b[:], getattr(T, nm)[:, :])
        setattr(Cn, nm, b)
    return Cn


def precast(K, T, Cn, which):
    with ExitStack() as es:
        stf = [K.sb(es, f"pc_f{i}", [128, 8, 2048], F32) for i in range(2)]
        stb = [K.sb(es, f"pc_b{i}", [128, 8, 2048], BF16) for i in range(2)]
        engs = ("act", "dve", "pool")
        n = [0]

        def one(src, dst, a, b):
            i = n[0] % 2
            K.D("sp", stf[i][:, 0:a, 0:b], src)
            for q in range(a):
                K.cp(engs[(n[0] + q) % 3], stb[i][:, q, 0:b], stf[i][:, q, 0:b])
            K.D("sp", dst, stb[i][:, 0:a, 0:b])
            n[0] += 1
        for name in which:
            src, dst = getattr(T, name), getattr(T, name + "_b")
            if name in ("w_eg", "w_eu", "w_ed"):
                sv = src.re("e (c p) n -> p (e c) n", p=128)
            else:
                sv = src.re("(c p) n -> p c n", p=128)
            dv = dst.re("(c p) n -> p c n", p=128)
            nchunk, ncol = sv.ap.shape[1], sv.ap.shape[2]
            cstep = max(1, min(nchunk, 16384 // min(ncol, 2048)))
            cstep = min(cstep, 8) if ncol >= 2048 else min(cstep, 32)
            for c0 in range(0, nchunk, cstep):
                a = min(cstep, nchunk - c0)
                for n0 in range(0, ncol, 2048):
                    b = min(2048, ncol - n0)
                    if ncol >= 2048:
                        one(sv[:, c0:c0 + a, n0:n0 + b], dv[:, c0:c0 + a, n0:n0 + b], a, b)
                    else:
                        i = n[0] % 2
                        fv = stf[i].re("p a b -> p (a b)")[:, 0:a * b].re("p (a b) -> p a b", a=a)
                        bv = stb[i].re("p a b -> p (a b)")[:, 0:a * b].re("p (a b) -> p a b", a=a)
                        K.D("sp", fv, sv[:, c0:c0 + a, n0:n0 + b])
                        K.cp(engs[n[0] % 3], bv, fv)
                        K.D("sp", dv[:, c0:c0 + a, n0:n0 + b], bv)
                        n[0] += 1


def phase_pw1(K, T, Cn):
    precast(K, T, Cn, ["w_in"])


def phase_pw2(K, T, Cn):
    import os
    lst = ["w_mem_kv", "w_o_cat", "w_out", "w_eg", "w_eu", "w_ed"]
    if os.environ.get("PW2"):
        lst = os.environ["PW2"].split(",")
    precast(K, T, Cn, lst)


def phase0(K, T, Cn):
    with ExitStack() as es:
        xin = [K.sb(es, f"p0x{i}", [128, 16, 512], F32) for i in range(2)]
        sq = [K.sb(es, f"p0sq{i}", [128, 512], F32) for i in range(2)]
        rs = [K.sb(es, f"p0rs{i}", [128, 512], F32) for i in range(2)]
        rs2 = [K.sb(es, f"p0rt{i}", [128, 512], F32) for i in range(2)]
        ao = [K.sb(es, f"p0a{i}", [128, 16, 512], BF16) for i in range(2)]
        pp = [K.ps(es, f"p0ps{i}", [128, 512]) for i in range(2)]
        xv = T.xT.re("(c p) t -> p c t", p=128)
        av = T.aT_d.re("(c p) t -> p c t", p=128)
        ng = NLOC // 512
        for g in range(ng):
            xb, ab, ps_ = xin[g % 2], ao[g % 2], pp[g % 2]
            sl = slice(g * 512, (g + 1) * 512)
            K.D("sp", xb[:, 0:8, :], xv[:, 0:8, sl])
            K.D("sp", xb[:, 8:16, :], xv[:, 8:16, sl])
            for c in range(16):
                s = sq[c % 2]
                K.act(s[:], xb[:, c, :], AF.Square)
                K.MM(ps_[:], Cn.ones[:], s[:], start=(c == 0), stop=(c == 15))
            K.act(rs[g % 2][:], ps_[:], AF.Sqrt, scale=1.0 / D_MODEL, bias=Cn.eps[:, 0:1])
            K.X("dve", "reciprocal", out=rs2[g % 2][:], in_=rs[g % 2][:])
            for c in range(16):
                K.stt(ab[:, c, :], xb[:, c, :], Cn.g_mix_c[:, c:c + 1], ALU.mult, rs2[g % 2][:], ALU.mult)
            K.D("sp", av[:, 0:8, sl], ab[:, 0:8, :])
            K.D("sp", av[:, 8:16, sl], ab[:, 8:16, :])


def phase1(K, T, Cn):
    jobs = []
    for p in range(3):
        jobs.append(("aq", C_AQ + 512 * p, 512, "FM", NOWN, 4 * p))
    for p in range(3):
        jobs.append(("ak", C_AK + 512 * p, 512, "FM", NSWA, 4 * p))
    for p in range(3):
        jobs.append(("av", C_AV + 512 * p, 512, "TM", NSWA, 512 * p))
    for p in range(3):
        jobs.append(("pre", C_BQ + 512 * p, 512, "FM", NOWN + 512, 4 * p))
    for p in range(6):
        jobs.append(("pre", C_BK + 512 * p, 512, "FM", NLOC, 12 + 4 * p))
    for p in range(3):
        jobs.append(("bz", C_BZ + 512 * p, 512, "TM", NOWN, 512 * p))
    jobs.append(("ba", C_BETA, 48, "TM", NLOC, 0))
    for p in range(2):
        jobs.append(("mq", C_MQ + 512 * p, 512, "FM", NOWN, 4 * p))
    import os
    if os.environ.get("P1JOBS"):
        jobs = [j for j in jobs if j[0] in os.environ["P1JOBS"].split(",")]
    work = []
    for sg in range(NLOC // 1024):
        for j in jobs:
            if sg * 1024 < j[4]:
                work.append((sg, j))
    work = sorted(work * int(os.environ.get("P1REP", "1")), key=lambda w_: w_[0])
    with ExitStack() as es:
        aT = [K.sb(es, f"aT{i}", [128, 16, 1024], BF16) for i in range(2)]
        W = [K.sb(es, f"W{i}", [128, 16, 512], BF16) for i in range(3)]
        rc = [K.sb(es, f"rc{i}", [128, 1024], F32) for i in range(2)]
        rsn = [K.sb(es, f"rsn{i}", [128, 1024], F32) for i in range(2)]
        pps = [K.ps(es, f"pp{i}", [128, 512]) for i in range(4)]
        rps = [K.ps(es, f"rp{i}", [128, 512]) for i in range(2)]
        qb = [K.sb(es, f"qb{i}", [128, 512], BF16) for i in range(2)]
        t1 = [K.sb(es, f"t1{i}", [128, 512], F32) for i in range(2)]
        qf = [K.sb(es, f"qf{i}", [128, 512], F32) for i in range(2)]
        t2 = [K.sb(es, f"t2{i}", [128, 512], F32) for i in range(2)]
        ob = [K.sb(es, f"ob{i}", [128, 512], BF16) for i in range(4)]
        obf = [K.sb(es, f"obf{i}", [128, 48], F32) for i in range(2)]
        wv = T.w_in_b.re("(c p) n -> p c n", p=128)
        av = T.aT_d.re("(c p) t -> p c t", p=128)
        ctr = {"ps": 0, "ob": 0, "rp": 0, "ev": 0}

        def load_w(i):
            sg, j = work[i]
            K.D("sp", W[i % 3][:, 0:8, 0:j[2]], wv[:, 0:8, j[1]:j[1] + j[2]])
            K.D("sp", W[i % 3][:, 8:16, 0:j[2]], wv[:, 8:16, j[1]:j[1] + j[2]])

        def load_a(sg):
            sl = slice(sg * 1024, (sg + 1) * 1024)
            K.D("sp", aT[sg % 2][:, 0:8, :], av[:, 0:8, sl])
            K.D("sp", aT[sg % 2][:, 8:16, :], av[:, 8:16, sl])
            if sg * 1024 < NSWA:
                K.D("sp", rc[sg % 2][:], T.c_ropeC[:, sl])
                K.D("sp", rsn[sg % 2][:], T.c_ropeS[:, sl])

        def evac(out, in_, func=AF.Copy, **kw):
            ctr["ev"] += 1
            if func == AF.Copy and ctr["ev"] % 2 == 0:
                K.cp("dve", out, in_)
            else:
                K.act(out, in_, func, **kw)

        def post_fm(sg, j, ct, th, ps_):
            name = j[0]
            tok0 = sg * 1024 + th * 512
            gt = j[5] + ct
            o = ob[ctr["ob"] % 4]
            ctr["ob"] += 1
            if name in ("aq", "ak"):
                q_, a_, b_ = qb[ctr["rp"] % 2], t1[ctr["rp"] % 2], t2[ctr["rp"] % 2]
                rp = rps[ctr["rp"] % 2]
                qf_ = qf[ctr["rp"] % 2]
                ctr["rp"] += 1
                K.act(qf_[:], ps_[:], AF.Copy)
                K.cp("dve", q_[:], qf_[:])
                K.MM(rp[:], Cn.perm_b[:], q_[:])
                lsl = slice(th * 512, (th + 1) * 512)
                K.tt("dve", a_[:], qf_[:], rc[sg % 2][:, lsl], ALU.mult)
                K.tt("dve", b_[:], rp[:], rsn[sg % 2][:, lsl], ALU.mult)
                dil = (1, 4, 16)[gt // 4]
                if os.environ.get("ROPEPLAIN"):
                    dil = 1
                dst = T.qa_d if name == "aq" else T.ka_d
                if dil == 1:
                    K.tt("pool", o[:], a_[:], b_[:], ALU.add)
                    K.D("sp", dst[gt, :, tok0:tok0 + 512], o[:])
                else:
                    K.tt("pool", o.re("p (r j) -> p r j", r=dil), a_.re("p (j r) -> p r j", r=dil),
                         b_.re("p (j r) -> p r j", r=dil), ALU.add)
                    nj = 512 // dil
                    K.D("sp", dst[gt].re("p (r j) -> p r j", r=dil)[:, :, tok0 // dil:tok0 // dil + nj],
                        o.re("p (r j) -> p r j", r=dil))
            elif name == "pre":
                evac(o[:], ps_[:])
                K.D("sp", T.pre_d[gt, :, tok0:tok0 + 512], o[:])
            elif name == "mq":
                evac(o[:], ps_[:])
                K.D("sp", T.mq_d[gt, :, tok0:tok0 + 512], o[:])
            elif name == "gate":
                K.act(o[:], ps_[:], AF.Sigmoid, bias=Cn.b_gate_c[:, gt:gt + 1])
                K.D("sp", T.gate_d[gt, :, tok0:tok0 + 512], o[:])

        def post_tm(sg, j, tt_, ps_):
            name = j[0]
            tok0 = sg * 1024 + tt_ * 128
            if name == "ba":
                o = obf[ctr["ob"] % 2]
                ctr["ob"] += 1
                evac(o[:], ps_[:, 0:48])
                K.D("sp", T.ba_d[tok0:tok0 + 128, :], o[:])
                return
            o = ob[ctr["ob"] % 4]
            ctr["ob"] += 1
            evac(o[:], ps_[:])
            dst = T.va_d if name == "av" else T.z_d
            K.D("sp", dst[tok0:tok0 + 128, j[5]:j[5] + 512], o[:])

        load_a(0)
        load_w(0)
        if len(work) > 1:
            load_w(1)
        cur_sg = -1
        for i, (sg, j) in enumerate(work):
            if sg != cur_sg:
                cur_sg = sg
                if (sg + 1) * 1024 < NLOC + 1 and sg + 1 < NLOC // 1024:
                    load_a(sg + 1)
            if i + 2 < len(work):
                load_w(i + 2)
            Wb, ab = W[i % 3], aT[sg % 2]
            ncols, tokmax = j[2], j[4]
            if j[3] == "FM":
                for ct in range(ncols // 128):
                    for th in range(2):
                        if sg * 1024 + th * 512 >= tokmax:
                            continue
                        ps_ = pps[ctr["ps"] % 4]
                        ctr["ps"] += 1
                        for c in range(16):
                            K.MM(ps_[:], Wb[:, c, ct * 128:(ct + 1) * 128], ab[:, c, th * 512:(th + 1) * 512],
                                 start=(c == 0), stop=(c == 15))
                        post_fm(sg, j, ct, th, ps_)
            else:
                for tt_ in range(8):
                    if sg * 1024 + tt_ * 128 >= tokmax:
                        continue
                    ps_ = pps[ctr["ps"] % 4]
                    ctr["ps"] += 1
                    for c in range(16):
                        K.MM(ps_[:, 0:ncols], ab[:, c, tt_ * 128:(tt_ + 1) * 128], Wb[:, c, 0:ncols],
                             start=(c == 0), stop=(c == 15))
                    post_tm(sg, j, tt_, ps_)


PHASES = []


def build(phases=None, debug=()):
    K = KB(debug=debug)
    T = Tens(K)
    with ExitStack() as es:
        Cn = load_consts(K, es, T)
        for name, fn in PHASES:
            if phases is None or name in phases:
                K.S.barrier()
                fn(K, T, Cn)
        K.S.finish()
    return K, T


def host_consts():
    i = np.arange(128)
    c = {}
    c["c_ident"] = np.eye(128, dtype=np.float32)
    c["c_ones"] = np.ones((128, 128), np.float32)
    row, col = i[:, None], i[None, :]
    c["c_triA"] = (row <= col).astype(np.float32)
    c["c_triB"] = (row >= col).astype(np.float32)
    c["c_mSA"] = np.where(col < row, 0.0, NEG).astype(np.float32)
    c["c_mSB"] = np.where(col > row, 0.0, NEG).astype(np.float32)
    c["c_mTA"] = np.where(col >= row, 0.0, NEG).astype(np.float32)
    c["c_mTB"] = np.where(col <= row, 0.0, NEG).astype(np.float32)
    sig = i.copy()
    sig[:16] = i[:16] + 16
    sig[16:32] = i[16:32] - 16
    perm = np.zeros((128, 128), np.float32)
    perm[sig, i] = 1.0
    c["c_perm"] = perm
    kk = np.arange(256)[None, :]
    p = i[:, None]
    band = np.where((kk >= p) & (kk <= p + 128), 0.0, NEG).astype(np.float32)
    band0 = np.where(kk < 64, NEG, band).astype(np.float32)
    c["c_band"] = np.ascontiguousarray(np.stack([band, band0], axis=1))
    return c


def rope_tables(half):
    idx = np.arange(NSWA)
    pos = idx if half == 0 else (SEQ - 1 - idx)
    pos = np.maximum(pos, 0)
    inv = (np.float32(500000.0) ** (-np.arange(16, dtype=np.float32) / np.float32(16))).astype(np.float32)
    ang = pos.astype(np.float32)[None, :] * inv[:, None]
    cos, sin = np.cos(ang).astype(np.float32), np.sin(ang).astype(np.float32)
    C = np.ones((128, NSWA), np.float32)
    S = np.zeros((128, NSWA), np.float32)
    C[0:16], C[16:32] = cos, cos
    S[0:16], S[16:32] = -sin, sin
    return C, S


def colmajor(v, w):
    return np.ascontiguousarray(np.asarray(v, np.float32).reshape(w, 128).T)


def prep_core(inp, core, names, consts):
    b, half = core // 2, core % 2
    m = {}
    for n in names:
        if n in consts:
            m[n] = consts[n]
        elif n == "xT":
            xb = inp["x"][b]
            m[n] = np.ascontiguousarray((xb if half == 0 else xb[::-1]).T)
        elif n == "memT":
            m[n] = np.ascontiguousarray(inp["mem"][b].T)
        elif n == "w_in":
            w = inp["w_in"][0]
            if half == 1:
                w = w.copy()
                for c0 in (C_BETA, C_ALPHA):
                    w[:, c0:c0 + 12], w[:, c0 + 12:c0 + 24] = inp["w_in"][0][:, c0 + 12:c0 + 24], inp["w_in"][0][:, c0:c0 + 12]
            m[n] = np.ascontiguousarray(w)
        elif n in ("g_mix_c", "g_mem_c", "g_ffn_c"):
            m[n] = colmajor(inp[n[:-2]][0], 16)
        elif n == "b_gate_c":
            m[n] = colmajor(inp["b_gate"][0].reshape(-1), 48)
        elif n == "convw":
            cw = inp["gdn_conv"][0]
            if half == 1:
                cw = cw[::-1]
            m[n] = np.ascontiguousarray(cw.reshape(5, 36, 128).transpose(2, 1, 0))
        elif n in ("alog_rep", "dtb_rep"):
            v = inp["gdn_a_log" if n == "alog_rep" else "gdn_dt_bias"][0]
            if half == 1:
                v = v[::-1]
            m[n] = np.ascontiguousarray(np.broadcast_to(v.reshape(1, 1, 24), (128, 64, 24))).astype(np.float32)
        elif n == "gnorm_rep":
            m[n] = np.ascontiguousarray(np.broadcast_to(inp["gdn_norm_g"][0][None, :], (128, 128)))
        elif n == "w_mem_kv":
            m[n] = inp["w_mem_kv"][0]
        elif n == "w_o_cat":
            m[n] = np.ascontiguousarray(np.concatenate([inp["w_o_swa"][0], inp["w_o_gdn"][0], inp["w_o_mem"][0]], axis=0))
        elif n == "w_out":
            m[n] = inp["w_out"][0]
        elif n == "w_route":
            m[n] = np.ascontiguousarray(np.concatenate([inp["w_route_group"][0], inp["w_route_expert"][0]], axis=1))
        elif n == "b_route_rep":
            m[n] = np.ascontiguousarray(np.broadcast_to(inp["b_route_group"][0][None, :], (128, 4)))
        elif n == "w_eg":
            m[n] = inp["w_expert_gate"][0]
        elif n == "w_eu":
            m[n] = inp["w_expert_up"][0]
        elif n == "w_ed":
            m[n] = inp["w_expert_down"][0]
        elif n == "g_final_rep":
            m[n] = np.ascontiguousarray(np.broadcast_to(inp["g_final"][None, :], (128, D_MODEL)))
        elif n in ("c_ropeC", "c_ropeS"):
            C, S = rope_tables(half)
            m[n] = C if n == "c_ropeC" else S
        else:
            raise KeyError(n)
        m[n] = np.ascontiguousarray(m[n], dtype=np.float32)
    return m


def run(inp, phases=None, debug=(), cores=8):
    K, T = build(phases, debug)
    consts = host_consts()
    inp = {k: np.asarray(v) for k, v in inp.items()}
    in_maps = [prep_core(inp, c, T.in_names, consts) for c in range(cores)]
    res = run_bass_kernel_spmd(K.nc, in_maps, core_ids=list(range(cores)))
    return res.results, K


def kernel(**inputs):
    results, _ = run(inputs)
    out = np.empty((4, SEQ, D_MODEL), np.float32)
    for c in range(8):
        b, half = c // 2, c % 2
        o = np.asarray(results[c]["out"], np.float32)
        if half == 0:
            out[b, :NOWN] = o
        else:
            out[b, NOWN:] = o[::-1]
    return out


def phase2(K, T, Cn):
    scale = float(HD) ** -0.5
    with ExitStack() as es:
        band = K.sb(es, "band", [128, 2, 256], F32)
        K.D("sp", band[:], T.c_band[:, :, :])
        NB = 2
        qT = [K.sb(es, f"s_q{i}", [128, NOWN], BF16) for i in range(NB)]
        kT = [K.sb(es, f"s_k{i}", [128, 64 + NOWN + 64], BF16) for i in range(NB)]
        vt = [K.sb(es, f"s_v{i}", [128, 33, 128], BF16) for i in range(NB)]
        ost = [K.sb(es, f"s_o{i}", [128, 32, 128], BF16) for i in range(NB)]
        lst = [K.sb(es, f"s_l{i}", [128, 32, 1], F32) for i in range(NB)]
        for i in range(NB):
            K.memset("pool", kT[i][:, 0:64], 0.0)
            K.memset("pool", vt[i][:, 0, :], 0.0)
        R = 3
        sm = [K.sb(es, f"s_sm{i}", [128, 256], F32) for i in range(R)]
        pb = [K.sb(es, f"s_p{i}", [128, 256], BF16) for i in range(R)]
        ptb = [K.sb(es, f"s_pt{i}", [128, 256], BF16) for i in range(R)]
        sml = [K.sb(es, f"s_st{i}", [128, 8], F32) for i in range(R)]
        ps_s = [K.ps(es, f"s_pss{i}", [128, 256]) for i in range(2)]
        ps_t = [K.ps(es, f"s_pst{i}", [128, 256], BF16) for i in range(2)]
        ps_o = [K.ps(es, f"s_pso{i}", [128, 128]) for i in range(2)]
        items = []
        for g, dil in enumerate((1, 4, 16)):
            for s_ in range(4):
                for r in range(dil):
                    items.append((g, dil, s_, r))
        cnt = [0]

        def load(it, i):
            g, dil, s_, r = it
            h = g * 4 + s_
            nq = NOWN // dil
            nb = nq // 128
            b = i % NB
            K.D("sp", qT[b][:, 0:nq], T.qa_d[h].re("p (r j) -> p r j", r=dil)[:, r, 0:nq])
            K.D("sp", kT[b][:, 64:64 + nq + 64], T.ka_d[h].re("p (r j) -> p r j", r=dil)[:, r, 0:nq + 64])
            vrow = T.va_d.re("(j r) c -> r j c", r=dil)[r]
            K.D("sp", vt[b][64:128, 0, :], vrow[0:64, h * 128:(h + 1) * 128])
            K.D("sp", vt[b][:, 1:nb + 1, :],
                vrow[64:64 + nb * 128, h * 128:(h + 1) * 128].re("(c p) x -> p c x", p=128))

        def stage_a(it, i, blk):
            k = cnt[0] % R
            b = i % NB
            pss = ps_s[cnt[0] % 2]
            K.MM(pss[:], qT[b][:, blk * 128:(blk + 1) * 128], kT[b][:, blk * 128:blk * 128 + 256])
            K.stt(sm[k][:], pss[:], scale, ALU.mult, band[:, 1 if blk == 0 else 0, :], ALU.add)
            K.X("dve", "reduce_max", out=sml[k][:, 0:1], in_=sm[k][:], axis=mybir.AxisListType.X)
            K.ts("dve", sml[k][:, 1:2], sml[k][:, 0:1], -1.0, ALU.mult)
            K.act(pb[k][:], sm[k][:], AF.Exp, bias=sml[k][:, 1:2], accum_out=sml[k][:, 2:3])
            cnt[0] += 1
            return k

        def stage_b(it, i, blk, k, c2):
            b = i % NB
            pst, pso = ps_t[c2 % 2], ps_o[c2 % 2]
            K.TR(pst[:, 0:128], pb[k][:, 0:128], Cn.ident_b[:])
            K.TR(pst[:, 128:256], pb[k][:, 128:256], Cn.ident_b[:])
            K.cp("act" if c2 % 2 else "dve", ptb[k][:], pst[:])
            K.MM(pso[:], ptb[k][:, 0:128], vt[b][:, blk, :], start=True, stop=False)
            K.MM(pso[:], ptb[k][:, 128:256], vt[b][:, blk + 1, :], start=False, stop=True)
            K.X("dve", "reciprocal", out=sml[k][:, 3:4], in_=sml[k][:, 2:3])
            K.ts("dve", ost[b][:, blk, :], pso[:], sml[k][:, 3:4], ALU.mult)
            K.act(sml[k][:, 4:5], sml[k][:, 2:3], AF.Ln)
            K.tt("dve", lst[b][:, blk, :], sml[k][:, 4:5], sml[k][:, 0:1], ALU.add)

        load(items[0], 0)
        c2 = 0
        for i, it in enumerate(items):
            g, dil, s_, r = it
            if i + 1 < len(items):
                load(items[i + 1], i + 1)
            nb = NOWN // dil // 128
            prev = None
            for blk in range(nb + 1):
                cur = None
                if blk < nb:
                    cur = stage_a(it, i, blk)
                if prev is not None:
                    stage_b(it, i, blk - 1, prev, c2)
                    c2 += 1
                prev = cur
            b = i % NB
            K.D("sp", T.oa_d[g].re("(j r) c -> r j c", r=dil)[r].re("(b p) c -> p b c", p=128)[:, :, s_ * 128:(s_ + 1) * 128],
                ost[b][:, 0:nb, :])
            K.D("sp", T.lse_d[g, s_].re("(j r) o -> r j o", r=dil)[r].re("(b p) o -> p b o", p=128),
                lst[b][:, 0:nb, :], slow=True)


def phase3(K, T, Cn):
    with ExitStack() as es:
        NB = 2
        og = [K.sb(es, f"c_o{i}", [128, 3, 512], BF16) for i in range(NB)]
        lg = [K.sb(es, f"c_l{i}", [128, 3, 4, 1], F32) for i in range(NB)]
        wk = [K.sb(es, f"c_w{i}", [128, 8, 4], F32) for i in range(NB)]
        al = [K.sb(es, f"c_a{i}", [128, 3, 4], F32) for i in range(NB)]
        acc = [K.sb(es, f"c_acc{i}", [128, 512], F32) for i in range(NB)]
        ab = [K.sb(es, f"c_ab{i}", [128, 512], BF16) for i in range(NB)]
        stg = [K.sb(es, f"c_st{i}", [128, 4, 512], BF16) for i in range(2)]
        pst = [K.ps(es, f"c_ps{i}", [128, 512], BF16) for i in range(2)]
        nt = NOWN // 128
        for t in range(nt):
            b = t % NB
            tok = slice(t * 128, (t + 1) * 128)
            K.D("sp", og[b][:], T.oa_d.re("g t c -> t g c")[tok])
            for g in range(3):
                K.D("sp", lg[b][:, g], T.lse_d[g].re("s t o -> t s o")[tok], slow=True)
            l3 = lg[b].re("p g s o -> p g (s o)")
            w = wk[b]
            K.tt("dve", w[:, 0, :], l3[:, 0, :], l3[:, 1, :], ALU.max)
            K.tt("dve", w[:, 0, :], w[:, 0, :], l3[:, 2, :], ALU.max)
            for g in range(3):
                K.tt("dve", w[:, 1 + g, :], l3[:, g, :], w[:, 0, :], ALU.subtract)
            K.act(w[:, 1:4, :], w[:, 1:4, :], AF.Exp)
            K.tt("dve", w[:, 4, :], w[:, 1, :], w[:, 2, :], ALU.add)
            K.tt("dve", w[:, 4, :], w[:, 4, :], w[:, 3, :], ALU.add)
            K.X("dve", "reciprocal", out=w[:, 5, :], in_=w[:, 4, :])
            for g in range(3):
                K.tt("dve", al[b][:, g, :], w[:, 1 + g, :], w[:, 5, :], ALU.mult)
            for s_ in range(4):
                cs = slice(s_ * 128, (s_ + 1) * 128)
                K.ts("dve", acc[b][:, cs], og[b][:, 0, cs], al[b][:, 0, s_:s_ + 1], ALU.mult)
                K.stt(acc[b][:, cs], og[b][:, 1, cs], al[b][:, 1, s_:s_ + 1], ALU.mult, acc[b][:, cs], ALU.add)
                K.stt(ab[b][:, cs], og[b][:, 2, cs], al[b][:, 2, s_:s_ + 1], ALU.mult, acc[b][:, cs], ALU.add)
            pp = pst[t % 2]
            for s_ in range(4):
                K.TR(pp[:, s_ * 128:(s_ + 1) * 128], ab[b][:, s_ * 128:(s_ + 1) * 128], Cn.ident_b[:])
            sg = stg[(t // 4) % 2]
            K.cp("act", sg[:, :, (t % 4) * 128:(t % 4 + 1) * 128], pp.re("p (s t) -> p s t", s=4))
            if t % 4 == 3:
                t0 = (t - 3) * 128
                K.D("sp", T.oT_d[0:4].re("c p t -> p c t")[:, :, t0:t0 + 512], sg[:])


def phase4(K, T, Cn):
    with ExitStack() as es:
        raw = K.sb(es, "g_raw", [128, 64, 48], F32)
        dtb = K.sb(es, "g_dtb", [128, 64, 24], F32)
        alog = K.sb(es, "g_alog", [128, 64, 24], F32)
        tmp = K.sb(es, "g_tmp", [128, 64, 24], F32)
        bg = K.sb(es, "g_bg", [128, 64, 72], F32)
        K.D("sp", raw[:], T.ba_d.re("(n p) c -> p n c", p=128))
        K.D("sp", dtb[:], T.dtb_rep[:, :, :])
        K.D("sp", alog[:], T.alog_rep[:, :, :])
        K.act(bg[:, :, 48:72], raw[:, :, 0:24], AF.Sigmoid)
        K.ts("dve", bg[:, :, 0:24], bg[:, :, 48:72], -1.0, ALU.mult)
        K.tt("dve", tmp[:], raw[:, :, 24:48], dtb[:], ALU.add)
        K.act(tmp[:], tmp[:], AF.Exp)
        K.act(tmp[:], tmp[:], AF.Ln, bias=Cn.onec[:, 0:1])
        K.act(alog[:], alog[:], AF.Exp)
        K.stt(bg[:, :, 24:48], tmp[:], -1.0, ALU.mult, alog[:], ALU.mult)
        K.D("sp", T.bg_d.re("(n p) c -> p n c", p=128), bg[:])
        cw = K.sb(es, "g_cw", [128, 36, 5], F32)
        K.D("sp", cw[:], T.convw[:, :, :])
        NB = 3
        xw = [K.sb(es, f"g_x{i}", [128, 516], BF16) for i in range(NB)]
        acc = [K.sb(es, f"g_acc{i}", [128, 512], F32) for i in range(2)]
        sl = [K.sb(es, f"g_s{i}", [128, 512], F32) for i in range(2)]
        sq = [K.sb(es, f"g_sq{i}", [128, 512], F32) for i in range(2)]
        rr = [K.sb(es, f"g_r{i}", [128, 512], F32) for i in range(2)]
        ob = [K.sb(es, f"g_ob{i}", [128, 512], BF16) for i in range(2)]
        tm = [K.sb(es, f"g_tm{i}", [128, 4, 128], BF16) for i in range(2)]
        pss = [K.ps(es, f"g_pss{i}", [128, 512]) for i in range(2)]
        pst = [K.ps(es, f"g_pst{i}", [128, 512], BF16) for i in range(2)]
        work = []
        for ct in range(36):
            nw = (NOWN if ct < 12 else NLOC) // 512
            for w in range(nw):
                work.append((ct, w))

        def load(i):
            ct, w = work[i]
            x = xw[i % NB]
            lo, hi = w * 512 - 2, w * 512 + 514
            a, b = max(lo, 0), min(hi, NLOC)
            if lo < 0:
                K.memset("pool", x[:, 0:2], 0.0)
            if hi > NLOC:
                K.memset("pool", x[:, 514:516], 0.0)
            K.D("sp", x[:, a - lo:b - lo], T.pre_d[ct, :, a:b])
        load(0)
        load(1)
        for i, (ct, w) in enumerate(work):
            if i + 2 < len(work):
                load(i + 2)
            x, a_, s_, q_, r_, o_ = xw[i % NB], acc[i % 2], sl[i % 2], sq[i % 2], rr[i % 2], ob[i % 2]
            K.ts("dve", a_[:], x[:, 0:512], cw[:, ct, 0:1], ALU.mult)
            for k in range(1, 5):
                K.stt(a_[:], x[:, k:k + 512], cw[:, ct, k:k + 1], ALU.mult, a_[:], ALU.add)
            kind, h = ct // 12, ct % 12
            tok = slice(w * 512, (w + 1) * 512)
            if kind < 2:
                K.act(s_[:], a_[:], AF.Silu)
                K.act(q_[:], s_[:], AF.Square)
                ps_ = pss[i % 2]
                K.MM(ps_[:], Cn.ones[:], q_[:])
                K.act(r_[:], ps_[:], AF.Sqrt, bias=Cn.eps[:, 0:1])
                K.X("dve", "reciprocal", out=r_[:], in_=r_[:])
                K.stt(o_[:], s_[:], (float(HD) ** -0.5) if kind == 0 else 1.0, ALU.mult, r_[:], ALU.mult)
                K.D("sp", (T.gq_d if kind == 0 else T.gk_d)[h, :, tok], o_[:])
            else:
                K.act(o_[:], a_[:], AF.Silu)
            if kind >= 1:
                pt, t_ = pst[i % 2], tm[i % 2]
                for j in range(4):
                    K.TR(pt[:, j * 128:(j + 1) * 128], o_[:, j * 128:(j + 1) * 128], Cn.ident_b[:])
                K.cp("act", t_[:], pt.re("p (n d) -> p n d", n=4))
                dst = T.gkt_d if kind == 1 else T.gv_d
                K.D("sp", dst[h].re("(n p) d -> p n d", p=128)[:, w * 4:(w + 1) * 4, :], t_[:])


def phase5(K, T, Cn):
    CH = 4
    NT_OWN, NT_ALL = NOWN // 128, NLOC // 128
    with ExitStack() as es:
        bg = K.sb(es, "r_bg", [128, 64, 72], F32)
        K.D("sp", bg[:], T.bg_d.re("(n p) c -> p n c", p=128))
        banks = [K.ps(es, f"r_ps{i}", [128, 512]) for i in range(8)]
        res = []
        for c in range(CH):
            r = NS()
            r.win = []
            for i in range(2):
                w = NS()
                w.kT = K.sb(es, f"r{c}_kT{i}", [128, 512], BF16)
                w.qT = K.sb(es, f"r{c}_qT{i}", [128, 512], BF16)
                w.kTM = K.sb(es, f"r{c}_kTM{i}", [128, 4, 128], BF16)
                w.vTM = K.sb(es, f"r{c}_vTM{i}", [128, 4, 128], BF16)
                w.ost = K.sb(es, f"r{c}_ost{i}", [128, 4, 128], F32)
                r.win.append(w)
            for nm in ("gbc", "Grow", "t1", "decS", "t2", "decT", "EG", "P0", "P1", "Q0", "Q1", "Y0", "Y1",
                       "attnT", "qeT", "ke", "nk", "vn", "S0", "S1"):
                setattr(r, nm, K.sb(es, f"r{c}_{nm}", [128, 128], F32))
            r.R = K.sb(es, f"r{c}_R", [128, 256], F32)
            r.g2 = K.sb(es, f"r{c}_g2", [128, 2], F32)
            r.gcc = K.sb(es, f"r{c}_gcc", [128, 2], F32)
            r.sm = K.sb(es, f"r{c}_sm", [128, 8], F32)
            r.k = 0
            r.q = 0
            res.append(r)

        def stage(c):
            r = res[c]
            r.k += 1
            r.q = 0

        def pslot(c):
            r = res[c]
            q = r.q
            r.q += 1
            assert q < 4
            return banks[2 * c + r.k % 2][:, q * 128:(q + 1) * 128]

        def tile_steps(c, d, h, n, w, li, has_out, sidx):
            r = res[c]
            tri, mS, mT = (Cn.triA, Cn.mSA, Cn.mTA) if d == 0 else (Cn.triB, Cn.mSB, Cn.mTB)
            L = 127 if d == 0 else 0
            col = d * 12 + h
            negb, gcol, beta = bg[:, n, col:col + 1], bg[:, n, 24 + col:25 + col], bg[:, n, 48 + col:49 + col]
            tsl = slice(li * 128, (li + 1) * 128)
            P, Q, Y = [r.P0, r.P1], [r.Q0, r.Q1], [r.Y0, r.Y1]
            S_old, S_new = (r.S0, r.S1) if sidx % 2 == 0 else (r.S1, r.S0)
            K.ts("dve", r.gbc[:], Cn.ones[:], gcol, ALU.mult)
            K.ts("dve", r.g2[:], Cn.ones[:, 0:2], gcol, ALU.mult)
            stage(c)
            p_g, p_c, p_kk = pslot(c), pslot(c), pslot(c)
            K.MM(p_g, r.gbc[:], tri[:])
            K.MM(p_c[:, 0:2], tri[:], r.g2[:])
            K.MM(p_kk, w.kT[:, tsl], w.kT[:, tsl])
            if has_out:
                p_qk = pslot(c)
                K.MM(p_qk, w.kT[:, tsl], w.qT[:, tsl])
            yield
            K.act(r.Grow[:], p_g, AF.Copy)
            K.act(r.gcc[:], p_c[:, 0:2], AF.Copy)
            yield
            K.stt(r.t1[:], r.Grow[:], r.gcc[:, 0:1], ALU.subtract, mS[:], ALU.subtract)
            K.act(r.decS[:], r.t1[:], AF.Exp, scale=-1.0)
            if has_out:
                K.stt(r.t2[:], r.Grow[:], r.gcc[:, 0:1], ALU.subtract, mT[:], ALU.add)
                K.act(r.decT[:], r.t2[:], AF.Exp)
                K.act(r.EG[:], r.Grow[:], AF.Exp)
            K.act(r.sm[:, 0:1], r.gcc[:, 0:1], AF.Exp)
            K.ts("dve", r.sm[:, 1:2], r.gcc[:, 0:1], r.Grow[:, L:L + 1], ALU.subtract)
            K.act(r.sm[:, 2:3], r.sm[:, 1:2], AF.Exp, scale=-1.0)
            K.act(r.sm[:, 3:4], r.Grow[:, L:L + 1], AF.Exp)
            K.tt("dve", r.sm[:, 4:5], negb, r.sm[:, 0:1], ALU.mult)
            yield
            K.stt(P[0][:], p_kk, negb, ALU.mult, r.decS[:], ALU.mult)
            if has_out:
                K.tt("dve", r.attnT[:], p_qk, r.decT[:], ALU.mult)
                K.tt("dve", r.qeT[:], w.qT[:, tsl], r.EG[:], ALU.mult)
            K.ts("dve", r.R[:, 0:128], w.vTM[:, li, :], beta, ALU.mult)
            K.ts("dve", r.R[:, 128:256], w.kTM[:, li, :], r.sm[:, 4:5], ALU.mult)
            K.ts("dve", r.ke[:], w.kTM[:, li, :], r.sm[:, 2:3], ALU.mult)
            stage(c)
            p_q0 = pslot(c)
            K.TR(p_q0, P[0][:], Cn.ident[:])
            yield
            K.act(Q[0][:], p_q0, AF.Copy)
            K.tt("dve", Y[0][:], Q[0][:], Cn.ident[:], ALU.add)
            yield
            for j in range(1, 7):
                pp, qp = P[(j - 1) % 2], Q[(j - 1) % 2]
                stage(c)
                pa = pslot(c)
                K.MM(pa, qp[:], pp[:])
                if j < 6:
                    pb_ = pslot(c)
                    K.MM(pb_, pp[:], qp[:])
                yield
                K.act(P[j % 2][:], pa, AF.Copy)
                if j < 6:
                    K.act(Q[j % 2][:], pb_, AF.Copy)
                yield
                stage(c)
                pc = pslot(c)
                K.MM(pc, P[j % 2][:], Y[(j - 1) % 2][:])
                yield
                K.tt("dve", Y[j % 2][:], pc, Y[(j - 1) % 2][:], ALU.add)
                yield
            Yf = Y[0]
            stage(c)
            p_k = pslot(c)
            K.MM(p_k, r.R[:, 128:256], Yf[:])
            yield
            K.act(r.nk[:], p_k, AF.Copy)
            yield
            stage(c)
            p_v = pslot(c)
            K.MM(p_v, Yf[:], r.R[:, 0:128], start=True, stop=False)
            K.MM(p_v, r.nk[:], S_old[:], start=False, stop=True)
            yield
            K.act(r.vn[:], p_v, AF.Copy)
            yield
            stage(c)
            if has_out:
                p_o = pslot(c)
                K.MM(p_o, r.qeT[:], S_old[:], start=True, stop=False)
                K.MM(p_o, r.attnT[:], r.vn[:], start=False, stop=True)
            p_s = pslot(c)
            K.MM(p_s, r.ke[:], r.vn[:])
            yield
            K.stt(S_new[:], S_old[:], r.sm[:, 3:4], ALU.mult, p_s, ALU.add)
            if has_out:
                K.cp("dve", w.ost[:, li, :], p_o)
            yield

        def chain(c, d, h):
            r = res[c]
            K.memset("pool", r.S0[:], 0.0)
            if d == 0:
                wins = [(w0, True) for w0 in range(0, NT_OWN, 4)]
            else:
                wins = [(w0, w0 < NT_OWN) for w0 in range(NT_ALL - 4, -1, -4)]

            def load(i):
                w0, ho = wins[i]
                w = r.win[i % 2]
                tk = slice(w0 * 128, (w0 + 4) * 128)
                K.D("sp", w.kT[:], T.gk_d[h, :, tk])
                if ho:
                    K.D("sp", w.qT[:], T.gq_d[h, :, tk])
                K.D("sp", w.kTM[:], T.gkt_d[h].re("(n p) x -> p n x", p=128)[:, w0:w0 + 4, :])
                K.D("sp", w.vTM[:], T.gv_d[h].re("(n p) x -> p n x", p=128)[:, w0:w0 + 4, :])
            load(0)
            sidx = 0
            for i, (w0, ho) in enumerate(wins):
                if i + 1 < len(wins):
                    load(i + 1)
                w = r.win[i % 2]
                order = range(4) if d == 0 else range(3, -1, -1)
                for li in order:
                    yield from tile_steps(c, d, h, w0 + li, w, li, ho, sidx)
                    sidx += 1
                if ho:
                    K.D("sp", T.ob_d[d].re("(n p) x -> p n x", p=128)[:, w0:w0 + 4, h * 128:(h + 1) * 128], w.ost[:])
                yield

        allc = [(d, h) for d in (1, 0) for h in range(12)]
        for b0 in range(0, len(allc), CH):
            gens = [chain(c, d, h) for c, (d, h) in enumerate(allc[b0:b0 + CH])]
            while gens:
                for g in list(gens):
                    try:
                        next(g)
                    except StopIteration:
                        gens.remove(g)


PHASES[:] = [("pw1", phase_pw1), ("p0", phase0), ("p1", phase1), ("p2", phase2), ("p3", phase3), ("p4", phase4), ("p5", phase5)]


def phase6(K, T, Cn):
    with ExitStack() as es:
        gn = K.sb(es, "o_gn", [128, 128], F32)
        K.D("sp", gn[:], T.gnorm_rep[:, :])
        oa = [K.sb(es, f"o_a{i}", [128, 1536], F32) for i in range(2)]
        obb = [K.sb(es, f"o_b{i}", [128, 1536], F32) for i in range(2)]
        zz = [K.sb(es, f"o_z{i}", [128, 1536], BF16) for i in range(2)]
        sz = [K.sb(es, f"o_sz{i}", [128, 1536], F32) for i in range(2)]
        on = [K.sb(es, f"o_n{i}", [128, 1536], F32) for i in range(2)]
        o16 = [K.sb(es, f"o_16{i}", [128, 1536], BF16) for i in range(2)]
        ss = [K.sb(es, f"o_ss{i}", [128, 16], F32) for i in range(2)]
        junk = K.sb(es, "o_junk", [128, 128], F32)
        stg = [K.sb(es, f"o_st{i}", [128, 12, 512], BF16) for i in range(2)]
        pst = [K.ps(es, f"o_ps{i}", [128, 512], BF16) for i in range(6)]
        for t in range(NOWN // 128):
            b = t % 2
            tok = slice(t * 128, (t + 1) * 128)
            K.D("sp", oa[b][:], T.ob_d[0, tok, :])
            K.D("sp", obb[b][:], T.ob_d[1, tok, :])
            K.D("sp", zz[b][:], T.z_d[tok, :])
            K.tt("dve", oa[b][:], oa[b][:], obb[b][:], ALU.add)
            for h in range(12):
                K.act(junk[:], oa[b][:, h * 128:(h + 1) * 128], AF.Square, accum_out=ss[b][:, h:h + 1])
            K.act(ss[b][:, 0:12], ss[b][:, 0:12], AF.Sqrt, scale=1.0 / HD, bias=Cn.eps[:, 0:1])
            K.X("dve", "reciprocal", out=ss[b][:, 0:12], in_=ss[b][:, 0:12])
            for h in range(12):
                hs = slice(h * 128, (h + 1) * 128)
                K.stt(on[b][:, hs], oa[b][:, hs], ss[b][:, h:h + 1], ALU.mult, gn[:], ALU.mult)
            K.act(sz[b][:], zz[b][:], AF.Silu)
            K.tt("dve", o16[b][:], on[b][:], sz[b][:], ALU.mult)
            sg = stg[(t // 4) % 2]
            for g in range(3):
                pp = pst[(t % 2) * 3 + g]
                for q in range(4):
                    h = g * 4 + q
                    K.TR(pp[:, q * 128:(q + 1) * 128], o16[b][:, h * 128:(h + 1) * 128], Cn.ident_b[:])
                K.cp("act" if g % 2 else "dve", sg[:, 4 * g:4 * g + 4, (t % 4) * 128:(t % 4 + 1) * 128],
                     pp.re("p (s t) -> p s t", s=4))
            if t % 4 == 3:
                t0 = (t - 3) * 128
                K.D("sp", T.oT_d[4:16].re("c p t -> p c t")[:, :, t0:t0 + 512], sg[:])


def phase7(K, T, Cn):
    scale = 256.0 ** -0.5
    with ExitStack() as es:
        mT = K.sb(es, "m_T", [128, 16, 256], F32)
        K.D("sp", mT[:], T.memT.re("(c p) m -> p c m", p=128))
        sq = [K.sb(es, f"m_sq{i}", [128, 256], F32) for i in range(2)]
        rs = K.sb(es, "m_rs", [128, 256], F32)
        mn = K.sb(es, "m_n", [128, 16, 256], BF16)
        Wkv = K.sb(es, "m_W", [128, 16, 2048], BF16)
        wv = T.w_mem_kv_b.re("(c p) n -> p c n", p=128)
        K.D("sp", Wkv[:, 0:8, :], wv[:, 0:8, :])
        K.D("sp", Wkv[:, 8:16, :], wv[:, 8:16, :])
        mkT = K.sb(es, "m_kT", [128, 8, 256], BF16)
        mv = K.sb(es, "m_v", [128, 2, 1024], BF16)
        pss = [K.ps(es, f"m_pss{i}", [128, 256]) for i in range(2)]
        psb = pss
        pst = [K.ps(es, f"m_pst{i}", [128, 256], BF16) for i in range(2)]
        pso = [K.ps(es, f"m_pso{i}", [128, 256]) for i in range(2)]
        for c in range(16):
            K.act(sq[c % 2][:], mT[:, c, :], AF.Square)
            K.MM(psb[0][:, 0:256], Cn.ones[:], sq[c % 2][:], start=(c == 0), stop=(c == 15))
        K.act(rs[:], psb[0][:, 0:256], AF.Sqrt, scale=1.0 / D_MODEL, bias=Cn.eps[:, 0:1])
        K.X("dve", "reciprocal", out=rs[:], in_=rs[:])
        for c in range(16):
            K.stt(mn[:, c, :], mT[:, c, :], Cn.g_mem_c[:, c:c + 1], ALU.mult, rs[:], ALU.mult)
        for ct in range(8):
            p_ = psb[ct % 2]
            for c in range(16):
                K.MM(p_[:, 0:256], Wkv[:, c, ct * 128:(ct + 1) * 128], mn[:, c, :], start=(c == 0), stop=(c == 15))
            K.cp("act", mkT[:, ct, :], p_[:, 0:256])
        for mt in range(2):
            for cb in range(4):
                p_ = psb[(mt * 4 + cb) % 2]
                for c in range(16):
                    K.MM(p_[:], mn[:, c, mt * 128:(mt + 1) * 128], Wkv[:, c, 1024 + cb * 256:1024 + (cb + 1) * 256],
                         start=(c == 0), stop=(c == 15))
                K.cp("dve", mv[:, mt, cb * 256:(cb + 1) * 256], p_[:])
        mq = [K.sb(es, f"m_q{i}", [128, 8, 512], BF16) for i in range(2)]
        pb = [K.sb(es, f"m_p{i}", [128, 256], BF16) for i in range(3)]
        ptb = [K.sb(es, f"m_pt{i}", [128, 256], BF16) for i in range(3)]
        sml = [K.sb(es, f"m_sm{i}", [128, 8], F32) for i in range(3)]
        om = [K.sb(es, f"m_o{i}", [128, 1024], BF16) for i in range(2)]
        stg = [K.sb(es, f"m_st{i}", [128, 8, 512], BF16) for i in range(2)]
        pso2 = [K.ps(es, f"m_pt2{i}", [128, 512], BF16) for i in range(2)]
        k = 0
        for t in range(NOWN // 128):
            if t % 4 == 0:
                K.D("sp", mq[(t // 4) % 2][:], T.mq_d.re("c p t -> p c t")[:, :, t * 128:t * 128 + 512])
            mqb = mq[(t // 4) % 2]
            tl = slice((t % 4) * 128, (t % 4 + 1) * 128)
            for h in range(4):
                kk = k % 3
                p_s, p_t, p_o = pss[k % 2], pst[k % 2], pso[k % 2]
                k += 1
                K.MM(p_s[:], mqb[:, 2 * h, tl], mkT[:, 2 * h, :], start=True, stop=False)
                K.MM(p_s[:], mqb[:, 2 * h + 1, tl], mkT[:, 2 * h + 1, :], start=False, stop=True)
                K.X("dve", "reduce_max", out=sml[kk][:, 0:1], in_=p_s[:], axis=mybir.AxisListType.X)
                K.ts("dve", sml[kk][:, 1:2], sml[kk][:, 0:1], -scale, ALU.mult)
                K.act(pb[kk][:], p_s[:], AF.Exp, bias=sml[kk][:, 1:2], scale=scale, accum_out=sml[kk][:, 2:3])
                K.TR(p_t[:, 0:128], pb[kk][:, 0:128], Cn.ident_b[:])
                K.TR(p_t[:, 128:256], pb[kk][:, 128:256], Cn.ident_b[:])
                K.cp("act" if k % 2 else "dve", ptb[kk][:], p_t[:])
                K.MM(p_o[:], ptb[kk][:, 0:128], mv[:, 0, h * 256:(h + 1) * 256], start=True, stop=False)
                K.MM(p_o[:], ptb[kk][:, 128:256], mv[:, 1, h * 256:(h + 1) * 256], start=False, stop=True)
                K.X("dve", "reciprocal", out=sml[kk][:, 3:4], in_=sml[kk][:, 2:3])
                K.ts("dve", om[t % 2][:, h * 256:(h + 1) * 256], p_o[:], sml[kk][:, 3:4], ALU.mult)
            sg = stg[(t // 4) % 2]
            for g in range(2):
                pp = pso2[g]
                for q in range(4):
                    K.TR(pp[:, q * 128:(q + 1) * 128], om[t % 2][:, (g * 4 + q) * 128:(g * 4 + q + 1) * 128], Cn.ident_b[:])
                K.cp("act" if g else "dve", sg[:, 4 * g:4 * g + 4, tl], pp.re("p (s t) -> p s t", s=4))
            if t % 4 == 3:
                t0 = (t - 3) * 128
                K.D("sp", T.oT_d[16:24].re("c p t -> p c t")[:, :, t0:t0 + 512], sg[:])


def phase8(K, T, Cn):
    with ExitStack() as es:
        oT = K.sb(es, "e_oT", [128, 24, 512], BF16)
        aT = K.sb(es, "e_aT", [128, 16, 512], BF16)
        xT = K.sb(es, "e_xT", [128, 16, 512], F32)
        Wo = [K.sb(es, f"e_Wo{i}", [128, 24, 128], BF16) for i in range(2)]
        Wg = [K.sb(es, f"e_Wg{i}", [128, 16, 3, 128], BF16) for i in range(2)]
        Wu = [K.sb(es, f"e_Wu{i}", [128, 16, 128], BF16) for i in range(2)]
        mix = K.sb(es, "e_mix", [128, 16, 512], BF16)
        xn = K.sb(es, "e_xn", [128, 16, 512], BF16)
        gs = [K.sb(es, f"e_g{i}", [128, 512], F32) for i in range(3)]
        ta = K.sb(es, "e_ta", [128, 512], F32)
        tb = K.sb(es, "e_tb", [128, 512], F32)
        sq = [K.sb(es, f"e_sq{i}", [128, 512], F32) for i in range(2)]
        rs = K.sb(es, "e_rs", [128, 512], F32)
        Wr = K.sb(es, "e_Wr", [128, 16, 36], F32)
        K.D("sp", Wr[:], T.w_route.re("(c p) n -> p c n", p=128))
        brr = K.sb(es, "e_br", [128, 4], F32)
        K.D("sp", brr[:], T.b_route_rep[:, :])
        lg = [K.sb(es, f"e_lg{i}", [128, 36], F32) for i in range(2)]
        rw = [K.sb(es, f"e_rw{i}", [128, 64], F32) for i in range(2)]
        wf = [K.sb(es, f"e_wf{i}", [128, 4, 8], F32) for i in range(2)]
        bk = [K.ps(es, f"e_bk{i}", [128, 512]) for i in range(8)]
        nb = [0]

        def bank():
            nb[0] += 1
            return bk[nb[0] % 8]
        wov = T.w_o_cat_b.re("(c p) n -> p c n", p=128)
        wiv = T.w_in_b.re("(c p) n -> p c n", p=128)
        wuv = T.w_out_b.re("(c p) n -> p c n", p=128)
        branches = ((0, 4), (4, 16), (16, 24))
        for tg in range(NOWN // 512):
            tk = slice(tg * 512, (tg + 1) * 512)
            K.D("sp", oT[:, 0:12, :], T.oT_d.re("c p t -> p c t")[:, 0:12, tk])
            K.D("sp", oT[:, 12:24, :], T.oT_d.re("c p t -> p c t")[:, 12:24, tk])
            K.D("sp", aT[:], T.aT_d.re("(c p) t -> p c t", p=128)[:, :, tk])
            K.D("sp", xT[:], T.xT.re("(c p) t -> p c t", p=128)[:, :, tk])

            def loadw(j):
                K.D("sp", Wo[j % 2][:], wov[:, :, j * 128:(j + 1) * 128])
                for br in range(3):
                    c0 = C_GATE + br * 2048 + j * 128
                    K.D("sp", Wg[j % 2][:, :, br, :], wiv[:, :, c0:c0 + 128])
            loadw(0)
            for j in range(16):
                if j + 1 < 16:
                    loadw(j + 1)
                py = []
                for (c0, c1) in branches:
                    p_ = bank()
                    for c in range(c0, c1):
                        K.MM(p_[:], Wo[j % 2][:, c, :], oT[:, c, :], start=(c == c0), stop=(c == c1 - 1))
                    py.append(p_)
                for br in range(3):
                    p_ = bank()
                    for c in range(16):
                        K.MM(p_[:], Wg[j % 2][:, c, br, :], aT[:, c, :], start=(c == 0), stop=(c == 15))
                    K.act(gs[br][:], p_[:], AF.Sigmoid, bias=Cn.b_gate_c[:, br * 16 + j:br * 16 + j + 1])
                K.tt("dve", ta[:], py[0][:], gs[0][:], ALU.mult)
                K.tt("dve", tb[:], py[1][:], gs[1][:], ALU.mult)
                K.tt("pool", ta[:], ta[:], tb[:], ALU.add)
                K.tt("dve", tb[:], py[2][:], gs[2][:], ALU.mult)
                K.tt("pool", mix[:, j, :], ta[:], tb[:], ALU.add)
            K.D("sp", Wu[0][:], wuv[:, :, 0:128])
            for j in range(16):
                if j + 1 < 16:
                    K.D("sp", Wu[(j + 1) % 2][:], wuv[:, :, (j + 1) * 128:(j + 2) * 128])
                p_ = bank()
                for c in range(16):
                    K.MM(p_[:], Wu[j % 2][:, c, :], mix[:, c, :], start=(c == 0), stop=(c == 15))
                K.tt("dve", xT[:, j, :], xT[:, j, :], p_[:], ALU.add)
            K.D("sp", T.x1T_d.re("c p t -> p c t")[:, :, tk], xT[:])
            pss_ = bank()
            for c in range(16):
                K.act(sq[c % 2][:], xT[:, c, :], AF.Square)
                K.MM(pss_[:], Cn.ones[:], sq[c % 2][:], start=(c == 0), stop=(c == 15))
            K.act(rs[:], pss_[:], AF.Sqrt, scale=1.0 / D_MODEL, bias=Cn.eps[:, 0:1])
            K.X("dve", "reciprocal", out=rs[:], in_=rs[:])
            for c in range(16):
                K.ts("dve" if c % 2 else "pool", xT[:, c, :], xT[:, c, :], Cn.g_ffn_c[:, c:c + 1], ALU.mult)
                K.tt("dve", xn[:, c, :], xT[:, c, :], rs[:], ALU.mult)
            K.D("sp", T.xnT_d.re("c p t -> p c t")[:, :, tk], xn[:])
            for t4 in range(4):
                tl = slice(t4 * 128, (t4 + 1) * 128)
                l_, r_, w_ = lg[t4 % 2], rw[t4 % 2], wf[t4 % 2]
                pl, pt = bank(), bank()
                for c in range(16):
                    K.MM(pl[:, 0:36], xT[:, c, tl], Wr[:, c, :], start=(c == 0), stop=(c == 15))
                K.TR(pt[:, 0:128], rs[:, tl], Cn.ident[:])
                K.cp("dve", r_[:, 0:1], pt[:, 0:1])
                K.ts("dve", l_[:], pl[:, 0:36], r_[:, 0:1], ALU.mult)
                K.tt("dve", l_[:, 0:4], l_[:, 0:4], brr[:], ALU.add)
                K.X("dve", "reduce_max", out=r_[:, 1:2], in_=l_[:, 0:4], axis=mybir.AxisListType.X)
                K.ts("dve", r_[:, 4:8], l_[:, 0:4], r_[:, 1:2], ALU.is_equal)
                K.ts("dve", r_[:, 2:3], r_[:, 1:2], -1.0, ALU.mult)
                K.act(r_[:, 8:12], l_[:, 0:4], AF.Exp, bias=r_[:, 2:3], accum_out=r_[:, 3:4])
                K.X("dve", "reciprocal", out=r_[:, 3:4], in_=r_[:, 3:4])
                K.ts("dve", r_[:, 16:24], l_[:, 4:12], r_[:, 4:5], ALU.mult)
                for g in range(1, 4):
                    K.stt(r_[:, 16:24], l_[:, 4 + 8 * g:12 + 8 * g], r_[:, 4 + g:5 + g], ALU.mult, r_[:, 16:24], ALU.add)
                K.X("dve", "reduce_max", out=r_[:, 12:13], in_=r_[:, 16:24], axis=mybir.AxisListType.X)
                K.ts("dve", r_[:, 24:32], r_[:, 16:24], r_[:, 12:13], ALU.is_equal)
                K.stt(r_[:, 32:40], r_[:, 24:32], NEG, ALU.mult, r_[:, 16:24], ALU.add)
                K.X("dve", "reduce_max", out=r_[:, 13:14], in_=r_[:, 32:40], axis=mybir.AxisListType.X)
                K.ts("dve", r_[:, 40:48], r_[:, 32:40], r_[:, 13:14], ALU.is_equal)
                K.tt("dve", r_[:, 14:15], r_[:, 13:14], r_[:, 12:13], ALU.subtract)
                K.act(r_[:, 14:15], r_[:, 14:15], AF.Exp)
                K.ts("dve", r_[:, 15:16], r_[:, 14:15], 1.0, ALU.add)
                K.X("dve", "reciprocal", out=r_[:, 15:16], in_=r_[:, 15:16])
                K.tt("dve", r_[:, 48:49], r_[:, 15:16], r_[:, 3:4], ALU.mult)
                K.tt("dve", r_[:, 49:50], r_[:, 48:49], r_[:, 14:15], ALU.mult)
                K.ts("dve", r_[:, 56:64], r_[:, 24:32], r_[:, 48:49], ALU.mult)
                K.stt(r_[:, 56:64], r_[:, 40:48], r_[:, 49:50], ALU.mult, r_[:, 56:64], ALU.add)
                for g in range(4):
                    K.ts("dve", w_[:, g, :], r_[:, 56:64], r_[:, 4 + g:5 + g], ALU.mult)
                K.D("sp", T.wgt_d[tg * 512 + t4 * 128:tg * 512 + (t4 + 1) * 128, :], w_.re("p g e -> p (g e)"))


def phase9(K, T, Cn):
    with ExitStack() as es:
        xn = K.sb(es, "f_xn", [128, 16, 512], BF16)
        x1 = K.sb(es, "f_x1", [128, 16, 512], F32)
        acc = K.sb(es, "f_acc", [128, 4, 2048], F32)
        wgt = K.sb(es, "f_wgt", [128, 4, 32], F32)
        Wg = [K.sb(es, f"f_Wg{i}", [128, 16, 512], BF16) for i in range(2)]
        Wu = [K.sb(es, f"f_Wu{i}", [128, 16, 512], BF16) for i in range(2)]
        Wd = K.sb(es, "f_Wd", [128, 4, 2048], BF16)
        hm = [K.sb(es, f"f_hm{i}", [128, 4, 512], BF16) for i in range(2)]
        sg = [K.sb(es, f"f_sg{i}", [128, 512], F32) for i in range(2)]
        gf = K.sb(es, "f_gf", [128, D_MODEL], F32)
        K.D("sp", gf[:], T.g_final_rep[:, :])
        sm = K.sb(es, "f_sm", [128, 8], F32)
        junk = K.sb(es, "f_junk", [128, 2048], BF16)
        bk = [K.ps(es, f"f_bk{i}", [128, 512]) for i in range(8)]
        nb = [0]

        def bank():
            nb[0] += 1
            return bk[nb[0] % 8]
        gv = T.w_eg_b.re("(e c p) n -> e p c n", c=16, p=128)
        uv = T.w_eu_b.re("(e c p) n -> e p c n", c=16, p=128)
        dv = T.w_ed_b.re("(e c p) n -> e p c n", c=4, p=128)
        for tg in range(NOWN // 512):
            tk = slice(tg * 512, (tg + 1) * 512)
            K.D("sp", xn[:], T.xnT_d.re("c p t -> p c t")[:, :, tk])
            K.D("sp", x1[:], T.x1T_d.re("c p t -> p c t")[:, :, tk])
            K.D("sp", wgt[:], T.wgt_d.re("(n p) e -> p n e", p=128)[:, tg * 4:(tg + 1) * 4, :])
            for t4 in range(4):
                for cg in range(4):
                    p_ = bank()
                    for q in range(4):
                        K.TR(p_[:, q * 128:(q + 1) * 128], x1[:, cg * 4 + q, t4 * 128:(t4 + 1) * 128], Cn.ident[:])
                    K.cp("act" if cg % 2 else "dve", acc[:, t4, cg * 512:(cg + 1) * 512], p_[:])

            def loadw(e):
                K.D("sp", Wg[e % 2][:], gv[e])
                K.D("sp", Wu[e % 2][:], uv[e])
            loadw(0)
            for e in range(32):
                if e + 1 < 32:
                    loadw(e + 1)
                K.D("sp", Wd[:], dv[e])
                h_ = hm[e % 2]
                for f in range(4):
                    pg, pu = bank(), bank()
                    for c in range(16):
                        K.MM(pg[:], Wg[e % 2][:, c, f * 128:(f + 1) * 128], xn[:, c, :], start=(c == 0), stop=(c == 15))
                    for c in range(16):
                        K.MM(pu[:], Wu[e % 2][:, c, f * 128:(f + 1) * 128], xn[:, c, :], start=(c == 0), stop=(c == 15))
                    K.act(sg[f % 2][:], pg[:], AF.Silu)
                    K.tt("dve", h_[:, f, :], sg[f % 2][:], pu[:], ALU.mult)
                for t4 in range(4):
                    for cb in range(4):
                        pd = bank()
                        for f in range(4):
                            K.MM(pd[:], h_[:, f, t4 * 128:(t4 + 1) * 128], Wd[:, f, cb * 512:(cb + 1) * 512],
                                 start=(f == 0), stop=(f == 3))
                        cs = slice(cb * 512, (cb + 1) * 512)
                        K.stt(acc[:, t4, cs], pd[:], wgt[:, t4, e:e + 1], ALU.mult, acc[:, t4, cs], ALU.add)
            ost = x1.re("p c t -> p (c t)").re("p (n d) -> p n d", n=4)
            for t4 in range(4):
                K.act(junk[:], acc[:, t4, :], AF.Square, accum_out=sm[:, t4:t4 + 1])
            K.act(sm[:, 0:4], sm[:, 0:4], AF.Sqrt, scale=1.0 / D_MODEL, bias=Cn.eps[:, 0:1])
            K.X("dve", "reciprocal", out=sm[:, 0:4], in_=sm[:, 0:4])
            for t4 in range(4):
                K.stt(ost[:, t4, :], acc[:, t4, :], sm[:, t4:t4 + 1], ALU.mult, gf[:], ALU.mult)
            K.D("sp", T.out.re("(n p) d -> p n d", p=128)[:, tg * 4:(tg + 1) * 4, :], ost)


PHASES[:] = [("pw1", phase_pw1), ("pw2", phase_pw2), ("p0", phase0), ("p1", phase1), ("p2", phase2), ("p3", phase3),
             ("p4", phase4), ("p5", phase5), ("p6", phase6), ("p7", phase7), ("p8", phase8), ("p9", phase9)]
```

```python
from contextlib import ExitStack
import numpy as np
import concourse.bass as bass
import concourse.mybir as mybir
from concourse.bass_utils import run_bass_kernel_spmd

F32 = mybir.dt.float32
BF16 = mybir.dt.bfloat16
AF = mybir.ActivationFunctionType
ALU = mybir.AluOpType

D_MODEL = 2048
SEQ = 8192
NLOC = 8192
NOWN = 4096
NSWA = 5120
HD = 128
IN_WIDTH = 17968
C_AQ, C_AK, C_AV, C_BQ, C_BK, C_BV, C_BZ, C_BETA, C_ALPHA, C_MQ, C_GATE = (
    0, 1536, 3072, 4608, 6144, 7680, 9216, 10752, 10776, 10800, 11824)
EPS = 1e-6
NEG = -1e30
POOL_ARITH = False


class V:
    __slots__ = ("buf", "ap")

    def __init__(self, buf, ap):
        self.buf = buf
        self.ap = ap

    def __getitem__(self, idx):
        return V(self.buf, self.ap[idx])

    def re(self, pat, **kw):
        return V(self.buf, self.ap.rearrange(pat, **kw))


class Buf:
    __slots__ = ("t", "w", "r", "name")

    def __init__(self, t, name=""):
        self.t = t
        self.w = None
        self.r = {}
        self.name = name

    def __getitem__(self, idx):
        return V(self, self.t[idx])

    def re(self, pat, **kw):
        return V(self, self.t[:].rearrange(pat, **kw))


class Sched:
    CAP = 30000

    def __init__(self, nc, ndma=48):
        self.nc = nc
        self.E = {"pe": nc.tensor, "act": nc.scalar, "dve": nc.vector, "pool": nc.gpsimd, "sp": nc.sync}
        self.cnt = {e: 0 for e in self.E}
        self.sems = {e: [] for e in self.E}
        self.seen = {e: {} for e in self.E}
        self.dsem = [nc.alloc_semaphore(f"dq{i}") for i in range(ndma)]
        self.dcnt = [0] * ndma
        self.dpool = {"sp": list(range(0, ndma - 16)), "pool": list(range(ndma - 16, ndma - 4)),
                      "act": list(range(ndma - 4, ndma))}
        self.drr = {q: 0 for q in self.dpool}
        self.nwait = 0
        self.ninst = 0

    def _sem(self, e, k):
        while len(self.sems[e]) <= k:
            self.sems[e].append(self.nc.alloc_semaphore(f"s_{e}_{len(self.sems[e])}"))
        return self.sems[e][k]

    def _wait(self, e, tok):
        if tok is None:
            return
        kind, a, c = tok
        if kind == "e":
            if a == e and e == "pe":
                return
            key = ("e", a)
            if self.seen[e].get(key, 0) >= c:
                return
            self.seen[e][key] = c
            k, v = (c - 1) // self.CAP, (c - 1) % self.CAP + 1
            self.E[e].wait_ge(self._sem(a, k), v)
        else:
            key = ("d", a)
            if self.seen[e].get(key, 0) >= c:
                return
            self.seen[e][key] = c
            self.E[e].wait_ge(self.dsem[a], 16 * c)
        self.nwait += 1

    def _deps(self, e, reads, writes):
        for b in reads:
            self._wait(e, b.w)
        for b in writes:
            self._wait(e, b.w)
            for t in list(b.r.values()):
                self._wait(e, t)

    def _commit(self, tok, key, reads, writes):
        for b in reads:
            b.r[key] = tok
        for b in writes:
            b.w = tok
            b.r = {}

    def op(self, e, fn, reads=(), writes=()):
        self._deps(e, reads, writes)
        inst = fn(self.E[e])
        self.cnt[e] += 1
        c = self.cnt[e]
        inst.then_inc(self._sem(e, (c - 1) // self.CAP), 1)
        self.ninst += 1
        tok = ("e", e, c)
        self._commit(tok, ("e", e), reads, writes)
        return tok

    def dma(self, q, out, in_, **kw):
        reads, writes = [in_.buf], [out.buf]
        self._deps(q, reads, writes)
        pl = self.dpool[q]
        i = pl[self.drr[q] % len(pl)]
        self.drr[q] += 1
        if self.dcnt[i] > 0:
            self._wait(q, ("d", i, self.dcnt[i]))
        inst = self.E[q].dma_start(out=out.ap, in_=in_.ap, **kw)
        self.dcnt[i] += 1
        assert self.dcnt[i] * 16 < 60000
        inst.then_inc(self.dsem[i], 16)
        self.ninst += 1
        tok = ("d", i, self.dcnt[i])
        self._commit(tok, ("d", i), reads, writes)
        return tok

    def barrier(self):
        for e in self.E:
            for a in self.E:
                if self.cnt[a]:
                    self._wait(e, ("e", a, self.cnt[a]))
            for i, c in enumerate(self.dcnt):
                if c:
                    self._wait(e, ("d", i, c))

    def finish(self):
        for i, c in enumerate(self.dcnt):
            if c:
                self._wait("sp", ("d", i, c))
        for e in self.E:
            if e != "sp" and self.cnt[e]:
                self._wait("sp", ("e", e, self.cnt[e]))


class KB:
    def __init__(self, debug=()):
        self.nc = bass.Bass("TRN2", target_bir_lowering=False)
        self.S = Sched(self.nc)
        self.debug = set(debug)
        self.outs = []

    def dram_in(self, name, shape, dtype=F32):
        return Buf(self.nc.dram_tensor(name, list(shape), dtype, kind="ExternalInput").ap(), name)

    def dram_out(self, name, shape, dtype=F32):
        self.outs.append(name)
        return Buf(self.nc.dram_tensor(name, list(shape), dtype, kind="ExternalOutput").ap(), name)

    def dram_tmp(self, name, shape, dtype):
        if name in self.debug:
            return self.dram_out(name, shape, dtype)
        return Buf(self.nc.dram_tensor(name, list(shape), dtype, kind="Internal").ap(), name)

    def sb(self, es, name, shape, dtype=F32):
        self.uid = getattr(self, "uid", 0) + 1
        name = f"{name}_u{self.uid}"
        return Buf(es.enter_context(self.nc.sbuf_tensor(name, list(shape), dtype)), name)

    def ps(self, es, name, shape, dtype=F32):
        self.uid = getattr(self, "uid", 0) + 1
        name = f"{name}_u{self.uid}"
        return Buf(es.enter_context(self.nc.psum_tensor(name, list(shape), dtype)), name)

    def X(self, e, meth, **kw):
        reads, writes, args = [], [], {}
        for k, v in kw.items():
            if isinstance(v, V):
                (writes if k in ("out", "accum_out") else reads).append(v.buf)
                args[k] = v.ap
            else:
                args[k] = v
        return self.S.op(e, lambda eng: getattr(eng, meth)(**args), reads, writes)

    def MM(self, out, lhsT, rhs, start=True, stop=True):
        return self.S.op("pe", lambda eng: eng.matmul(out.ap, lhsT=lhsT.ap, rhs=rhs.ap, start=start, stop=stop),
                         [lhsT.buf, rhs.buf], [out.buf])

    def TR(self, out, in_, ident):
        return self.S.op("pe", lambda eng: eng.transpose(out.ap, in_.ap, ident.ap), [in_.buf, ident.buf], [out.buf])

    def D(self, q, out, in_, slow=False):
        if slow:
            return self.S.dma(q, out, in_, allow_slow_non_contiguous=True)
        return self.S.dma(q, out, in_)

    def act(self, out, in_, func, **kw):
        return self.X("act", "activation", out=out, in_=in_, func=func, **kw)

    def tt(self, e, out, in0, in1, op):
        if e == "pool" and not POOL_ARITH:
            e = "dve"
        return self.X(e, "tensor_tensor", out=out, in0=in0, in1=in1, op=op)

    def ts(self, e, out, in0, scalar1, op0, scalar2=None, op1=None):
        if e == "pool" and not POOL_ARITH:
            e = "dve"
        kw = dict(out=out, in0=in0, scalar1=scalar1, scalar2=scalar2, op0=op0)
        if op1 is not None:
            kw["op1"] = op1
        return self.X(e, "tensor_scalar", **kw)

    def stt(self, out, in0, scalar, op0, in1, op1):
        return self.X("dve", "scalar_tensor_tensor", out=out, in0=in0, scalar=scalar, op0=op0, in1=in1, op1=op1)

    def memset(self, e, out, val):
        return self.S.op(e, lambda eng: eng.memset(out.ap, val), [], [out.buf])

    def cp(self, e, out, in_):
        if e == "act":
            return self.act(out, in_, AF.Copy)
        return self.X(e, "tensor_copy", out=out, in_=in_)


class NS:
    pass


def tensor_specs():
    sp = {}

    def di(name, shape):
        sp[name] = ("in", shape, F32)

    def dt(name, shape, dtype):
        sp[name] = ("tmp", shape, dtype)
    di("xT", [D_MODEL, NLOC])
    di("memT", [D_MODEL, 256])
    di("w_in", [D_MODEL, IN_WIDTH])
    di("g_mix_c", [128, 16])
    di("g_mem_c", [128, 16])
    di("g_ffn_c", [128, 16])
    di("b_gate_c", [128, 48])
    di("convw", [128, 36, 5])
    di("alog_rep", [128, 64, 24])
    di("dtb_rep", [128, 64, 24])
    di("gnorm_rep", [128, 128])
    di("w_mem_kv", [D_MODEL, 2048])
    di("w_o_cat", [3072, D_MODEL])
    di("w_out", [D_MODEL, D_MODEL])
    di("w_route", [D_MODEL, 36])
    di("b_route_rep", [128, 4])
    di("w_eg", [32, D_MODEL, 512])
    di("w_eu", [32, D_MODEL, 512])
    di("w_ed", [32, 512, D_MODEL])
    di("g_final_rep", [128, D_MODEL])
    for nm in ("ident", "ones", "triA", "triB", "mSA", "mSB", "mTA", "mTB", "perm"):
        di("c_" + nm, [128, 128])
    di("c_ropeC", [128, NSWA])
    di("c_ropeS", [128, NSWA])
    di("c_band", [128, 2, 256])
    dt("w_in_b", [D_MODEL, IN_WIDTH], BF16)
    dt("w_mem_kv_b", [D_MODEL, 2048], BF16)
    dt("w_o_cat_b", [3072, D_MODEL], BF16)
    dt("w_out_b", [D_MODEL, D_MODEL], BF16)
    dt("w_eg_b", [32 * D_MODEL, 512], BF16)
    dt("w_eu_b", [32 * D_MODEL, 512], BF16)
    dt("w_ed_b", [32 * 512, D_MODEL], BF16)
    dt("aT_d", [D_MODEL, NLOC], BF16)
    dt("qa_d", [12, 128, NOWN], BF16)
    dt("ka_d", [12, 128, NSWA], BF16)
    dt("va_d", [NSWA, 1536], BF16)
    dt("pre_d", [36, 128, NLOC], BF16)
    dt("z_d", [NOWN, 1536], BF16)
    dt("ba_d", [NLOC, 48], F32)
    dt("mq_d", [8, 128, NOWN], BF16)
    dt("gate_d", [48, 128, NOWN], BF16)
    dt("oa_d", [3, NOWN, 512], BF16)
    dt("lse_d", [3, 4, NOWN, 1], F32)
    dt("oT_d", [24, 128, NOWN], BF16)
    dt("gq_d", [12, 128, NOWN], BF16)
    dt("gk_d", [12, 128, NLOC], BF16)
    dt("gkt_d", [12, NLOC, 128], BF16)
    dt("gv_d", [12, NLOC, 128], BF16)
    dt("ob_d", [2, NOWN, 1536], F32)
    dt("bg_d", [NLOC, 72], F32)
    dt("x1T_d", [16, 128, NOWN], F32)
    dt("xnT_d", [16, 128, NOWN], BF16)
    dt("wgt_d", [NOWN, 32], F32)
    sp["out"] = ("out", [NOWN, D_MODEL], F32)
    return sp


class Tens:
    def __init__(self, K):
        object.__setattr__(self, "_K", K)
        object.__setattr__(self, "_sp", tensor_specs())
        object.__setattr__(self, "in_names", [])

    def __getattr__(self, name):
        kind, shape, dtype = self._sp[name]
        K = self._K
        if kind == "in":
            b = K.dram_in(name, shape, dtype)
            self.in_names.append(name)
        elif kind == "out":
            b = K.dram_out(name, shape, dtype)
        else:
            b = K.dram_tmp(name, shape, dtype)
        object.__setattr__(self, name, b)
        return b


def load_consts(K, es, T):
    Cn = NS()
    for nm in ("ident", "ones", "triA", "triB", "mSA", "mSB", "mTA", "mTB"):
        b = K.sb(es, "k_" + nm, [128, 128], F32)
        K.D("sp", b[:], getattr(T, "c_" + nm)[:, :])
        setattr(Cn, nm, b)
    Cn.ident_b = K.sb(es, "k_ident_b", [128, 128], BF16)
    K.cp("dve", Cn.ident_b[:], Cn.ident[:])
    Cn.ones_b = K.sb(es, "k_ones_b", [128, 128], BF16)
    K.cp("dve", Cn.ones_b[:], Cn.ones[:])
    permf = K.sb(es, "k_perm_f", [128, 128], F32)
    K.D("sp", permf[:], T.c_perm[:, :])
    Cn.perm_b = K.sb(es, "k_perm_b", [128, 128], BF16)
    K.cp("dve", Cn.perm_b[:], permf[:])
    Cn.eps = K.sb(es, "k_eps", [128, 2], F32)
    K.memset("dve", Cn.eps[:], EPS)
    Cn.onec = K.sb(es, "k_onec", [128, 2], F32)
    K.memset("dve", Cn.onec[:], 1.0)
    for nm, w in (("g_mix_c", 16), ("g_mem_c", 16), ("g_ffn_c", 16), ("b_gate_c", 48)):
        b = K.sb(es, "k_" + nm, [128, w], F32)
        K.D("sp", b[:], getattr(T, nm)[:, :])
        setattr(Cn, nm, b)
    return Cn


def precast(K, T, Cn, which):
    with ExitStack() as es:
        stf = [K.sb(es, f"pc_f{i}", [128, 8, 2048], F32) for i in range(2)]
        stb = [K.sb(es, f"pc_b{i}", [128, 8, 2048], BF16) for i in range(2)]
        engs = ("act", "dve", "pool")
        n = [0]

        def one(src, dst, a, b):
            i = n[0] % 2
            K.D("sp", stf[i][:, 0:a, 0:b], src)
            for q in range(a):
                K.cp(engs[(n[0] + q) % 3], stb[i][:, q, 0:b], stf[i][:, q, 0:b])
            K.D("sp", dst, stb[i][:, 0:a, 0:b])
            n[0] += 1
        for name in which:
            src, dst = getattr(T, name), getattr(T, name + "_b")
            if name in ("w_eg", "w_eu", "w_ed"):
                sv = src.re("e (c p) n -> p (e c) n", p=128)
            else:
                sv = src.re("(c p) n -> p c n", p=128)
            dv = dst.re("(c p) n -> p c n", p=128)
            nchunk, ncol = sv.ap.shape[1], sv.ap.shape[2]
            cstep = max(1, min(nchunk, 16384 // min(ncol, 2048)))
            cstep = min(cstep, 8) if ncol >= 2048 else min(cstep, 32)
            for c0 in range(0, nchunk, cstep):
                a = min(cstep, nchunk - c0)
                for n0 in range(0, ncol, 2048):
                    b = min(2048, ncol - n0)
                    if ncol >= 2048:
                        one(sv[:, c0:c0 + a, n0:n0 + b], dv[:, c0:c0 + a, n0:n0 + b], a, b)
                    else:
                        i = n[0] % 2
                        fv = stf[i].re("p a b -> p (a b)")[:, 0:a * b].re("p (a b) -> p a b", a=a)
                        bv = stb[i].re("p a b -> p (a b)")[:, 0:a * b].re("p (a b) -> p a b", a=a)
                        K.D("sp", fv, sv[:, c0:c0 + a, n0:n0 + b])
                        K.cp(engs[n[0] % 3], bv, fv)
                        K.D("sp", dv[:, c0:c0 + a, n0:n0 + b], bv)
                        n[0] += 1


def phase_pw1(K, T, Cn):
    precast(K, T, Cn, ["w_in"])


def phase_pw2(K, T, Cn):
    import os
    lst = ["w_mem_kv", "w_o_cat", "w_out", "w_eg", "w_eu", "w_ed"]
    if os.environ.get("PW2"):
        lst = os.environ["PW2"].split(",")
    precast(K, T, Cn, lst)


def phase0(K, T, Cn):
    with ExitStack() as es:
        xin = [K.sb(es, f"p0x{i}", [128, 16, 512], F32) for i in range(2)]
        sq = [K.sb(es, f"p0sq{i}", [128, 512], F32) for i in range(2)]
        rs = [K.sb(es, f"p0rs{i}", [128, 512], F32) for i in range(2)]
        rs2 = [K.sb(es, f"p0rt{i}", [128, 512], F32) for i in range(2)]
        ao = [K.sb(es, f"p0a{i}", [128, 16, 512], BF16) for i in range(2)]
        pp = [K.ps(es, f"p0ps{i}", [128, 512]) for i in range(2)]
        xv = T.xT.re("(c p) t -> p c t", p=128)
        av = T.aT_d.re("(c p) t -> p c t", p=128)
        ng = NLOC // 512
        for g in range(ng):
            xb, ab, ps_ = xin[g % 2], ao[g % 2], pp[g % 2]
            sl = slice(g * 512, (g + 1) * 512)
            K.D("sp", xb[:, 0:8, :], xv[:, 0:8, sl])
            K.D("sp", xb[:, 8:16, :], xv[:, 8:16, sl])
            for c in range(16):
                s = sq[c % 2]
                K.act(s[:], xb[:, c, :], AF.Square)
                K.MM(ps_[:], Cn.ones[:], s[:], start=(c == 0), stop=(c == 15))
            K.act(rs[g % 2][:], ps_[:], AF.Sqrt, scale=1.0 / D_MODEL, bias=Cn.eps[:, 0:1])
            K.X("dve", "reciprocal", out=rs2[g % 2][:], in_=rs[g % 2][:])
            for c in range(16):
                K.stt(ab[:, c, :], xb[:, c, :], Cn.g_mix_c[:, c:c + 1], ALU.mult, rs2[g % 2][:], ALU.mult)
            K.D("sp", av[:, 0:8, sl], ab[:, 0:8, :])
            K.D("sp", av[:, 8:16, sl], ab[:, 8:16, :])


def phase1(K, T, Cn):
    jobs = []
    for p in range(3):
        jobs.append(("aq", C_AQ + 512 * p, 512, "FM", NOWN, 4 * p))
    for p in range(3):
        jobs.append(("ak", C_AK + 512 * p, 512, "FM", NSWA, 4 * p))
    for p in range(3):
        jobs.append(("av", C_AV + 512 * p, 512, "TM", NSWA, 512 * p))
    for p in range(3):
        jobs.append(("pre", C_BQ + 512 * p, 512, "FM", NOWN + 512, 4 * p))
    for p in range(6):
        jobs.append(("pre", C_BK + 512 * p, 512, "FM", NLOC, 12 + 4 * p))
    for p in range(3):
        jobs.append(("bz", C_BZ + 512 * p, 512, "TM", NOWN, 512 * p))
    jobs.append(("ba", C_BETA, 48, "TM", NLOC, 0))
    for p in range(2):
        jobs.append(("mq", C_MQ + 512 * p, 512, "FM", NOWN, 4 * p))
    import os
    if os.environ.get("P1JOBS"):
        jobs = [j for j in jobs if j[0] in os.environ["P1JOBS"].split(",")]
    work = []
    for sg in range(NLOC // 1024):
        for j in jobs:
            if sg * 1024 < j[4]:
                work.append((sg, j))
    work = sorted(work * int(os.environ.get("P1REP", "1")), key=lambda w_: w_[0])
    with ExitStack() as es:
        aT = [K.sb(es, f"aT{i}", [128, 16, 1024], BF16) for i in range(2)]
        W = [K.sb(es, f"W{i}", [128, 16, 512], BF16) for i in range(3)]
        rc = [K.sb(es, f"rc{i}", [128, 1024], F32) for i in range(2)]
        rsn = [K.sb(es, f"rsn{i}", [128, 1024], F32) for i in range(2)]
        pps = [K.ps(es, f"pp{i}", [128, 512]) for i in range(4)]
        rps = [K.ps(es, f"rp{i}", [128, 512]) for i in range(2)]
        qb = [K.sb(es, f"qb{i}", [128, 512], BF16) for i in range(2)]
        t1 = [K.sb(es, f"t1{i}", [128, 512], F32) for i in range(2)]
        qf = [K.sb(es, f"qf{i}", [128, 512], F32) for i in range(2)]
        t2 = [K.sb(es, f"t2{i}", [128, 512], F32) for i in range(2)]
        ob = [K.sb(es, f"ob{i}", [128, 512], BF16) for i in range(4)]
        obf = [K.sb(es, f"obf{i}", [128, 48], F32) for i in range(2)]
        wv = T.w_in_b.re("(c p) n -> p c n", p=128)
        av = T.aT_d.re("(c p) t -> p c t", p=128)
        ctr = {"ps": 0, "ob": 0, "rp": 0, "ev": 0}

        def load_w(i):
            sg, j = work[i]
            K.D("sp", W[i % 3][:, 0:8, 0:j[2]], wv[:, 0:8, j[1]:j[1] + j[2]])
            K.D("sp", W[i % 3][:, 8:16, 0:j[2]], wv[:, 8:16, j[1]:j[1] + j[2]])

        def load_a(sg):
            sl = slice(sg * 1024, (sg + 1) * 1024)
            K.D("sp", aT[sg % 2][:, 0:8, :], av[:, 0:8, sl])
            K.D("sp", aT[sg % 2][:, 8:16, :], av[:, 8:16, sl])
            if sg * 1024 < NSWA:
                K.D("sp", rc[sg % 2][:], T.c_ropeC[:, sl])
                K.D("sp", rsn[sg % 2][:], T.c_ropeS[:, sl])

        def evac(out, in_, func=AF.Copy, **kw):
            ctr["ev"] += 1
            if func == AF.Copy and ctr["ev"] % 2 == 0:
                K.cp("dve", out, in_)
            else:
                K.act(out, in_, func, **kw)

        def post_fm(sg, j, ct, th, ps_):
            name = j[0]
            tok0 = sg * 1024 + th * 512
            gt = j[5] + ct
            o = ob[ctr["ob"] % 4]
            ctr["ob"] += 1
            if name in ("aq", "ak"):
                q_, a_, b_ = qb[ctr["rp"] % 2], t1[ctr["rp"] % 2], t2[ctr["rp"] % 2]
                rp = rps[ctr["rp"] % 2]
                qf_ = qf[ctr["rp"] % 2]
                ctr["rp"] += 1
                K.act(qf_[:], ps_[:], AF.Copy)
                K.cp("dve", q_[:], qf_[:])
                K.MM(rp[:], Cn.perm_b[:], q_[:])
                lsl = slice(th * 512, (th + 1) * 512)
                K.tt("dve", a_[:], qf_[:], rc[sg % 2][:, lsl], ALU.mult)
                K.tt("dve", b_[:], rp[:], rsn[sg % 2][:, lsl], ALU.mult)
                dil = (1, 4, 16)[gt // 4]
                if os.environ.get("ROPEPLAIN"):
                    dil = 1
                dst = T.qa_d if name == "aq" else T.ka_d
                if dil == 1:
                    K.tt("pool", o[:], a_[:], b_[:], ALU.add)
                    K.D("sp", dst[gt, :, tok0:tok0 + 512], o[:])
                else:
                    K.tt("pool", o.re("p (r j) -> p r j", r=dil), a_.re("p (j r) -> p r j", r=dil),
                         b_.re("p (j r) -> p r j", r=dil), ALU.add)
                    nj = 512 // dil
                    K.D("sp", dst[gt].re("p (r j) -> p r j", r=dil)[:, :, tok0 // dil:tok0 // dil + nj],
                        o.re("p (r j) -> p r j", r=dil))
            elif name == "pre":
                evac(o[:], ps_[:])
                K.D("sp", T.pre_d[gt, :, tok0:tok0 + 512], o[:])
            elif name == "mq":
                evac(o[:], ps_[:])
                K.D("sp", T.mq_d[gt, :, tok0:tok0 + 512], o[:])
            elif name == "gate":
                K.act(o[:], ps_[:], AF.Sigmoid, bias=Cn.b_gate_c[:, gt:gt + 1])
                K.D("sp", T.gate_d[gt, :, tok0:tok0 + 512], o[:])

        def post_tm(sg, j, tt_, ps_):
            name = j[0]
            tok0 = sg * 1024 + tt_ * 128
            if name == "ba":
                o = obf[ctr["ob"] % 2]
                ctr["ob"] += 1
                evac(o[:], ps_[:, 0:48])
                K.D("sp", T.ba_d[tok0:tok0 + 128, :], o[:])
                return
            o = ob[ctr["ob"] % 4]
            ctr["ob"] += 1
            evac(o[:], ps_[:])
            dst = T.va_d if name == "av" else T.z_d
            K.D("sp", dst[tok0:tok0 + 128, j[5]:j[5] + 512], o[:])

        load_a(0)
        load_w(0)
        if len(work) > 1:
            load_w(1)
        cur_sg = -1
        for i, (sg, j) in enumerate(work):
            if sg != cur_sg:
                cur_sg = sg
                if (sg + 1) * 1024 < NLOC + 1 and sg + 1 < NLOC // 1024:
                    load_a(sg + 1)
            if i + 2 < len(work):
                load_w(i + 2)
            Wb, ab = W[i % 3], aT[sg % 2]
            ncols, tokmax = j[2], j[4]
            if j[3] == "FM":
                for ct in range(ncols // 128):
                    for th in range(2):
                        if sg * 1024 + th * 512 >= tokmax:
                            continue
                        ps_ = pps[ctr["ps"] % 4]
                        ctr["ps"] += 1
                        for c in range(16):
                            K.MM(ps_[:], Wb[:, c, ct * 128:(ct + 1) * 128], ab[:, c, th * 512:(th + 1) * 512],
                                 start=(c == 0), stop=(c == 15))
                        post_fm(sg, j, ct, th, ps_)
            else:
                for tt_ in range(8):
                    if sg * 1024 + tt_ * 128 >= tokmax:
                        continue
                    ps_ = pps[ctr["ps"] % 4]
                    ctr["ps"] += 1
                    for c in range(16):
                        K.MM(ps_[:, 0:ncols], ab[:, c, tt_ * 128:(tt_ + 1) * 128], Wb[:, c, 0:ncols],
                             start=(c == 0), stop=(c == 15))
                    post_tm(sg, j, tt_, ps_)


PHASES = []


def build(phases=None, debug=()):
    K = KB(debug=debug)
    T = Tens(K)
    with ExitStack() as es:
        Cn = load_consts(K, es, T)
        for name, fn in PHASES:
            if phases is None or name in phases:
                K.S.barrier()
                fn(K, T, Cn)
        K.S.finish()
    return K, T


def host_consts():
    i = np.arange(128)
    c = {}
    c["c_ident"] = np.eye(128, dtype=np.float32)
    c["c_ones"] = np.ones((128, 128), np.float32)
    row, col = i[:, None], i[None, :]
    c["c_triA"] = (row <= col).astype(np.float32)
    c["c_triB"] = (row >= col).astype(np.float32)
    c["c_mSA"] = np.where(col < row, 0.0, NEG).astype(np.float32)
    c["c_mSB"] = np.where(col > row, 0.0, NEG).astype(np.float32)
    c["c_mTA"] = np.where(col >= row, 0.0, NEG).astype(np.float32)
    c["c_mTB"] = np.where(col <= row, 0.0, NEG).astype(np.float32)
    sig = i.copy()
    sig[:16] = i[:16] + 16
    sig[16:32] = i[16:32] - 16
    perm = np.zeros((128, 128), np.float32)
    perm[sig, i] = 1.0
    c["c_perm"] = perm
    kk = np.arange(256)[None, :]
    p = i[:, None]
    band = np.where((kk >= p) & (kk <= p + 128), 0.0, NEG).astype(np.float32)
    band0 = np.where(kk < 64, NEG, band).astype(np.float32)
    c["c_band"] = np.ascontiguousarray(np.stack([band, band0], axis=1))
    return c


def rope_tables(half):
    idx = np.arange(NSWA)
    pos = idx if half == 0 else (SEQ - 1 - idx)
    pos = np.maximum(pos, 0)
    inv = (np.float32(500000.0) ** (-np.arange(16, dtype=np.float32) / np.float32(16))).astype(np.float32)
    ang = pos.astype(np.float32)[None, :] * inv[:, None]
    cos, sin = np.cos(ang).astype(np.float32), np.sin(ang).astype(np.float32)
    C = np.ones((128, NSWA), np.float32)
    S = np.zeros((128, NSWA), np.float32)
    C[0:16], C[16:32] = cos, cos
    S[0:16], S[16:32] = -sin, sin
    return C, S


def colmajor(v, w):
    return np.ascontiguousarray(np.asarray(v, np.float32).reshape(w, 128).T)


def prep_core(inp, core, names, consts):
    b, half = core // 2, core % 2
    m = {}
    for n in names:
        if n in consts:
            m[n] = consts[n]
        elif n == "xT":
            xb = inp["x"][b]
            m[n] = np.ascontiguousarray((xb if half == 0 else xb[::-1]).T)
        elif n == "memT":
            m[n] = np.ascontiguousarray(inp["mem"][b].T)
        elif n == "w_in":
            w = inp["w_in"][0]
            if half == 1:
                w = w.copy()
                for c0 in (C_BETA, C_ALPHA):
                    w[:, c0:c0 + 12], w[:, c0 + 12:c0 + 24] = inp["w_in"][0][:, c0 + 12:c0 + 24], inp["w_in"][0][:, c0:c0 + 12]
            m[n] = np.ascontiguousarray(w)
        elif n == "g_mix_c":
            m[n] = colmajor(inp["g_mix"][0], 16)
        elif n == "g_mem_c":
            m[n] = colmajor(inp["g_mem"][0], 16)
        elif n == "g_ffn_c":
            m[n] = colmajor(inp["g_ffn"][0], 16)
        elif n == "b_gate_c":
            m[n] = colmajor(inp["b_gate"][0].reshape(-1), 48)
        elif n == "convw":
            cw = inp["gdn_conv"][0]
            if half == 1:
                cw = cw[::-1]
            m[n] = np.ascontiguousarray(cw.reshape(5, 36, 128).transpose(2, 1, 0))
        elif n in ("alog_rep", "dtb_rep"):
            v = inp["gdn_a_log" if n == "alog_rep" else "gdn_dt_bias"][0]
            if half == 1:
                v = v[::-1]
            m[n] = np.ascontiguousarray(np.broadcast_to(v.reshape(1, 1, 24), (128, 64, 24))).astype(np.float32)
        elif n == "gnorm_rep":
            m[n] = np.ascontiguousarray(np.broadcast_to(inp["gdn_norm_g"][0][None, :], (128, 128)))
        elif n == "w_mem_kv":
            m[n] = inp["w_mem_kv"][0]
        elif n == "w_o_cat":
            m[n] = np.ascontiguousarray(np.concatenate([inp["w_o_swa"][0], inp["w_o_gdn"][0], inp["w_o_mem"][0]], axis=0))
        elif n == "w_out":
            m[n] = inp["w_out"][0]
        elif n == "w_route":
            m[n] = np.ascontiguousarray(np.concatenate([inp["w_route_group"][0], inp["w_route_expert"][0]], axis=1))
        elif n == "b_route_rep":
            m[n] = np.ascontiguousarray(np.broadcast_to(inp["b_route_group"][0][None, :], (128, 4)))
        elif n == "w_eg":
            m[n] = inp["w_expert_gate"][0]
        elif n == "w_eu":
            m[n] = inp["w_expert_up"][0]
        elif n == "w_ed":
            m[n] = inp["w_expert_down"][0]
        elif n == "g_final_rep":
            m[n] = np.ascontiguousarray(np.broadcast_to(inp["g_final"][None, :], (128, D_MODEL)))
        elif n in ("c_ropeC", "c_ropeS"):
            C, S = rope_tables(half)
            m[n] = C if n == "c_ropeC" else S
        else:
            raise KeyError(n)
        m[n] = np.ascontiguousarray(m[n], dtype=np.float32)
    return m


def run(inp, phases=None, debug=(), cores=8):
    K, T = build(phases, debug)
    consts = host_consts()
    inp = {k: np.asarray(v) for k, v in inp.items()}
    in_maps = [prep_core(inp, c, T.in_names, consts) for c in range(cores)]
    res = run_bass_kernel_spmd(K.nc, in_maps, core_ids=list(range(cores)))
    return res.results, K


def kernel(**inputs):
    results, _ = run(inputs)
    out = np.empty((4, SEQ, D_MODEL), np.float32)
    for c in range(8):
        b, half = c // 2, c % 2
        o = np.asarray(results[c]["out"], np.float32)
        if half == 0:
            out[b, :NOWN] = o
        else:
            out[b, NOWN:] = o[::-1]
    return out


def phase2(K, T, Cn):
    scale = float(HD) ** -0.5
    with ExitStack() as es:
        band = K.sb(es, "band", [128, 2, 256], F32)
        K.D("sp", band[:], T.c_band[:, :, :])
        NB = 2
        qT = [K.sb(es, f"s_q{i}", [128, NOWN], BF16) for i in range(NB)]
        kT = [K.sb(es, f"s_k{i}", [128, 64 + NOWN + 64], BF16) for i in range(NB)]
        vt = [K.sb(es, f"s_v{i}", [128, 33, 128], BF16) for i in range(NB)]
        ost = [K.sb(es, f"s_o{i}", [128, 32, 128], BF16) for i in range(NB)]
        lst = [K.sb(es, f"s_l{i}", [128, 32, 1], F32) for i in range(NB)]
        for i in range(NB):
            K.memset("pool", kT[i][:, 0:64], 0.0)
            K.memset("pool", vt[i][:, 0, :], 0.0)
        R = 3
        sm = [K.sb(es, f"s_sm{i}", [128, 256], F32) for i in range(R)]
        pb = [K.sb(es, f"s_p{i}", [128, 256], BF16) for i in range(R)]
        ptb = [K.sb(es, f"s_pt{i}", [128, 256], BF16) for i in range(R)]
        sml = [K.sb(es, f"s_st{i}", [128, 8], F32) for i in range(R)]
        ps_s = [K.ps(es, f"s_pss{i}", [128, 256]) for i in range(2)]
        ps_t = [K.ps(es, f"s_pst{i}", [128, 256], BF16) for i in range(2)]
        ps_o = [K.ps(es, f"s_pso{i}", [128, 128]) for i in range(2)]
        items = []
        for g, dil in enumerate((1, 4, 16)):
            for s_ in range(4):
                for r in range(dil):
                    items.append((g, dil, s_, r))
        cnt = [0]

        def load(it, i):
            g, dil, s_, r = it
            h = g * 4 + s_
            nq = NOWN // dil
            nb = nq // 128
            b = i % NB
            K.D("sp", qT[b][:, 0:nq], T.qa_d[h].re("p (r j) -> p r j", r=dil)[:, r, 0:nq])
            K.D("sp", kT[b][:, 64:64 + nq + 64], T.ka_d[h].re("p (r j) -> p r j", r=dil)[:, r, 0:nq + 64])
            vrow = T.va_d.re("(j r) c -> r j c", r=dil)[r]
            K.D("sp", vt[b][64:128, 0, :], vrow[0:64, h * 128:(h + 1) * 128])
            K.D("sp", vt[b][:, 1:nb + 1, :],
                vrow[64:64 + nb * 128, h * 128:(h + 1) * 128].re("(c p) x -> p c x", p=128))

        def stage_a(it, i, blk):
            k = cnt[0] % R
            b = i % NB
            pss = ps_s[cnt[0] % 2]
            K.MM(pss[:], qT[b][:, blk * 128:(blk + 1) * 128], kT[b][:, blk * 128:blk * 128 + 256])
            K.stt(sm[k][:], pss[:], scale, ALU.mult, band[:, 1 if blk == 0 else 0, :], ALU.add)
            K.X("dve", "reduce_max", out=sml[k][:, 0:1], in_=sm[k][:], axis=mybir.AxisListType.X)
            K.ts("dve", sml[k][:, 1:2], sml[k][:, 0:1], -1.0, ALU.mult)
            K.act(pb[k][:], sm[k][:], AF.Exp, bias=sml[k][:, 1:2], accum_out=sml[k][:, 2:3])
            cnt[0] += 1
            return k

        def stage_b(it, i, blk, k, c2):
            b = i % NB
            pst, pso = ps_t[c2 % 2], ps_o[c2 % 2]
            K.TR(pst[:, 0:128], pb[k][:, 0:128], Cn.ident_b[:])
            K.TR(pst[:, 128:256], pb[k][:, 128:256], Cn.ident_b[:])
            K.cp("act" if c2 % 2 else "dve", ptb[k][:], pst[:])
            K.MM(pso[:], ptb[k][:, 0:128], vt[b][:, blk, :], start=True, stop=False)
            K.MM(pso[:], ptb[k][:, 128:256], vt[b][:, blk + 1, :], start=False, stop=True)
            K.X("dve", "reciprocal", out=sml[k][:, 3:4], in_=sml[k][:, 2:3])
            K.ts("dve", ost[b][:, blk, :], pso[:], sml[k][:, 3:4], ALU.mult)
            K.act(sml[k][:, 4:5], sml[k][:, 2:3], AF.Ln)
            K.tt("dve", lst[b][:, blk, :], sml[k][:, 4:5], sml[k][:, 0:1], ALU.add)

        load(items[0], 0)
        c2 = 0
        for i, it in enumerate(items):
            g, dil, s_, r = it
            if i + 1 < len(items):
                load(items[i + 1], i + 1)
            nb = NOWN // dil // 128
            prev = None
            for blk in range(nb + 1):
                cur = None
                if blk < nb:
                    cur = stage_a(it, i, blk)
                if prev is not None:
                    stage_b(it, i, blk - 1, prev, c2)
                    c2 += 1
                prev = cur
            b = i % NB
            K.D("sp", T.oa_d[g].re("(j r) c -> r j c", r=dil)[r].re("(b p) c -> p b c", p=128)[:, :, s_ * 128:(s_ + 1) * 128],
                ost[b][:, 0:nb, :])
            K.D("sp", T.lse_d[g, s_].re("(j r) o -> r j o", r=dil)[r].re("(b p) o -> p b o", p=128),
                lst[b][:, 0:nb, :], slow=True)


def phase3(K, T, Cn):
    with ExitStack() as es:
        NB = 2
        og = [K.sb(es, f"c_o{i}", [128, 3, 512], BF16) for i in range(NB)]
        lg = [K.sb(es, f"c_l{i}", [128, 3, 4, 1], F32) for i in range(NB)]
        wk = [K.sb(es, f"c_w{i}", [128, 8, 4], F32) for i in range(NB)]
        al = [K.sb(es, f"c_a{i}", [128, 3, 4], F32) for i in range(NB)]
        acc = [K.sb(es, f"c_acc{i}", [128, 512], F32) for i in range(NB)]
        ab = [K.sb(es, f"c_ab{i}", [128, 512], BF16) for i in range(NB)]
        stg = [K.sb(es, f"c_st{i}", [128, 4, 512], BF16) for i in range(2)]
        pst = [K.ps(es, f"c_ps{i}", [128, 512], BF16) for i in range(2)]
        nt = NOWN // 128
        for t in range(nt):
            b = t % NB
            tok = slice(t * 128, (t + 1) * 128)
            K.D("sp", og[b][:], T.oa_d.re("g t c -> t g c")[tok])
            for g in range(3):
                K.D("sp", lg[b][:, g], T.lse_d[g].re("s t o -> t s o")[tok], slow=True)
            l3 = lg[b].re("p g s o -> p g (s o)")
            w = wk[b]
            K.tt("dve", w[:, 0, :], l3[:, 0, :], l3[:, 1, :], ALU.max)
            K.tt("dve", w[:, 0, :], w[:, 0, :], l3[:, 2, :], ALU.max)
            for g in range(3):
                K.tt("dve", w[:, 1 + g, :], l3[:, g, :], w[:, 0, :], ALU.subtract)
            K.act(w[:, 1:4, :], w[:, 1:4, :], AF.Exp)
            K.tt("dve", w[:, 4, :], w[:, 1, :], w[:, 2, :], ALU.add)
            K.tt("dve", w[:, 4, :], w[:, 4, :], w[:, 3, :], ALU.add)
            K.X("dve", "reciprocal", out=w[:, 5, :], in_=w[:, 4, :])
            for g in range(3):
                K.tt("dve", al[b][:, g, :], w[:, 1 + g, :], w[:, 5, :], ALU.mult)
            for s_ in range(4):
                cs = slice(s_ * 128, (s_ + 1) * 128)
                K.ts("dve", acc[b][:, cs], og[b][:, 0, cs], al[b][:, 0, s_:s_ + 1], ALU.mult)
                K.stt(acc[b][:, cs], og[b][:, 1, cs], al[b][:, 1, s_:s_ + 1], ALU.mult, acc[b][:, cs], ALU.add)
                K.stt(ab[b][:, cs], og[b][:, 2, cs], al[b][:, 2, s_:s_ + 1], ALU.mult, acc[b][:, cs], ALU.add)
            pp = pst[t % 2]
            for s_ in range(4):
                K.TR(pp[:, s_ * 128:(s_ + 1) * 128], ab[b][:, s_ * 128:(s_ + 1) * 128], Cn.ident_b[:])
            sg = stg[(t // 4) % 2]
            K.cp("act", sg[:, :, (t % 4) * 128:(t % 4 + 1) * 128], pp.re("p (s t) -> p s t", s=4))
            if t % 4 == 3:
                t0 = (t - 3) * 128
                K.D("sp", T.oT_d[0:4].re("c p t -> p c t")[:, :, t0:t0 + 512], sg[:])


def phase4(K, T, Cn):
    with ExitStack() as es:
        raw = K.sb(es, "g_raw", [128, 64, 48], F32)
        dtb = K.sb(es, "g_dtb", [128, 64, 24], F32)
        alog = K.sb(es, "g_alog", [128, 64, 24], F32)
        tmp = K.sb(es, "g_tmp", [128, 64, 24], F32)
        bg = K.sb(es, "g_bg", [128, 64, 72], F32)
        K.D("sp", raw[:], T.ba_d.re("(n p) c -> p n c", p=128))
        K.D("sp", dtb[:], T.dtb_rep[:, :, :])
        K.D("sp", alog[:], T.alog_rep[:, :, :])
        K.act(bg[:, :, 48:72], raw[:, :, 0:24], AF.Sigmoid)
        K.ts("dve", bg[:, :, 0:24], bg[:, :, 48:72], -1.0, ALU.mult)
        K.tt("dve", tmp[:], raw[:, :, 24:48], dtb[:], ALU.add)
        K.act(tmp[:], tmp[:], AF.Exp)
        K.act(tmp[:], tmp[:], AF.Ln, bias=Cn.onec[:, 0:1])
        K.act(alog[:], alog[:], AF.Exp)
        K.stt(bg[:, :, 24:48], tmp[:], -1.0, ALU.mult, alog[:], ALU.mult)
        K.D("sp", T.bg_d.re("(n p) c -> p n c", p=128), bg[:])
        cw = K.sb(es, "g_cw", [128, 36, 5], F32)
        K.D("sp", cw[:], T.convw[:, :, :])
        NB = 3
        xw = [K.sb(es, f"g_x{i}", [128, 516], BF16) for i in range(NB)]
        acc = [K.sb(es, f"g_acc{i}", [128, 512], F32) for i in range(2)]
        sl = [K.sb(es, f"g_s{i}", [128, 512], F32) for i in range(2)]
        sq = [K.sb(es, f"g_sq{i}", [128, 512], F32) for i in range(2)]
        rr = [K.sb(es, f"g_r{i}", [128, 512], F32) for i in range(2)]
        ob = [K.sb(es, f"g_ob{i}", [128, 512], BF16) for i in range(2)]
        tm = [K.sb(es, f"g_tm{i}", [128, 4, 128], BF16) for i in range(2)]
        pss = [K.ps(es, f"g_pss{i}", [128, 512]) for i in range(2)]
        pst = [K.ps(es, f"g_pst{i}", [128, 512], BF16) for i in range(2)]
        work = []
        for ct in range(36):
            nw = (NOWN if ct < 12 else NLOC) // 512
            for w in range(nw):
                work.append((ct, w))

        def load(i):
            ct, w = work[i]
            x = xw[i % NB]
            lo, hi = w * 512 - 2, w * 512 + 514
            a, b = max(lo, 0), min(hi, NLOC)
            if lo < 0:
                K.memset("pool", x[:, 0:2], 0.0)
            if hi > NLOC:
                K.memset("pool", x[:, 514:516], 0.0)
            K.D("sp", x[:, a - lo:b - lo], T.pre_d[ct, :, a:b])
        load(0)
        load(1)
        for i, (ct, w) in enumerate(work):
            if i + 2 < len(work):
                load(i + 2)
            x, a_, s_, q_, r_, o_ = xw[i % NB], acc[i % 2], sl[i % 2], sq[i % 2], rr[i % 2], ob[i % 2]
            K.ts("dve", a_[:], x[:, 0:512], cw[:, ct, 0:1], ALU.mult)
            for k in range(1, 5):
                K.stt(a_[:], x[:, k:k + 512], cw[:, ct, k:k + 1], ALU.mult, a_[:], ALU.add)
            kind, h = ct // 12, ct % 12
            tok = slice(w * 512, (w + 1) * 512)
            if kind < 2:
                K.act(s_[:], a_[:], AF.Silu)
                K.act(q_[:], s_[:], AF.Square)
                ps_ = pss[i % 2]
                K.MM(ps_[:], Cn.ones[:], q_[:])
                K.act(r_[:], ps_[:], AF.Sqrt, bias=Cn.eps[:, 0:1])
                K.X("dve", "reciprocal", out=r_[:], in_=r_[:])
                K.stt(o_[:], s_[:], (float(HD) ** -0.5) if kind == 0 else 1.0, ALU.mult, r_[:], ALU.mult)
                K.D("sp", (T.gq_d if kind == 0 else T.gk_d)[h, :, tok], o_[:])
            else:
                K.act(o_[:], a_[:], AF.Silu)
            if kind >= 1:
                pt, t_ = pst[i % 2], tm[i % 2]
                for j in range(4):
                    K.TR(pt[:, j * 128:(j + 1) * 128], o_[:, j * 128:(j + 1) * 128], Cn.ident_b[:])
                K.cp("act", t_[:], pt.re("p (n d) -> p n d", n=4))
                dst = T.gkt_d if kind == 1 else T.gv_d
                K.D("sp", dst[h].re("(n p) d -> p n d", p=128)[:, w * 4:(w + 1) * 4, :], t_[:])


def phase5(K, T, Cn):
    CH = 4
    NT_OWN, NT_ALL = NOWN // 128, NLOC // 128
    with ExitStack() as es:
        bg = K.sb(es, "r_bg", [128, 64, 72], F32)
        K.D("sp", bg[:], T.bg_d.re("(n p) c -> p n c", p=128))
        banks = [K.ps(es, f"r_ps{i}", [128, 512]) for i in range(8)]
        res = []
        for c in range(CH):
            r = NS()
            r.win = []
            for i in range(2):
                w = NS()
                w.kT = K.sb(es, f"r{c}_kT{i}", [128, 512], BF16)
                w.qT = K.sb(es, f"r{c}_qT{i}", [128, 512], BF16)
                w.kTM = K.sb(es, f"r{c}_kTM{i}", [128, 4, 128], BF16)
                w.vTM = K.sb(es, f"r{c}_vTM{i}", [128, 4, 128], BF16)
                w.ost = K.sb(es, f"r{c}_ost{i}", [128, 4, 128], F32)
                r.win.append(w)
            for nm in ("gbc", "Grow", "t1", "decS", "t2", "decT", "EG", "P0", "P1", "Q0", "Q1", "Y0", "Y1",
                       "attnT", "qeT", "ke", "nk", "vn", "S0", "S1"):
                setattr(r, nm, K.sb(es, f"r{c}_{nm}", [128, 128], F32))
            r.R = K.sb(es, f"r{c}_R", [128, 256], F32)
            r.g2 = K.sb(es, f"r{c}_g2", [128, 2], F32)
            r.gcc = K.sb(es, f"r{c}_gcc", [128, 2], F32)
            r.sm = K.sb(es, f"r{c}_sm", [128, 8], F32)
            r.k = 0
            r.q = 0
            res.append(r)

        def stage(c):
            r = res[c]
            r.k += 1
            r.q = 0

        def pslot(c):
            r = res[c]
            q = r.q
            r.q += 1
            assert q < 4
            return banks[2 * c + r.k % 2][:, q * 128:(q + 1) * 128]

        def tile_steps(c, d, h, n, w, li, has_out, sidx):
            r = res[c]
            tri, mS, mT = (Cn.triA, Cn.mSA, Cn.mTA) if d == 0 else (Cn.triB, Cn.mSB, Cn.mTB)
            L = 127 if d == 0 else 0
            col = d * 12 + h
            negb, gcol, beta = bg[:, n, col:col + 1], bg[:, n, 24 + col:25 + col], bg[:, n, 48 + col:49 + col]
            tsl = slice(li * 128, (li + 1) * 128)
            P, Q, Y = [r.P0, r.P1], [r.Q0, r.Q1], [r.Y0, r.Y1]
            S_old, S_new = (r.S0, r.S1) if sidx % 2 == 0 else (r.S1, r.S0)
            K.ts("dve", r.gbc[:], Cn.ones[:], gcol, ALU.mult)
            K.ts("dve", r.g2[:], Cn.ones[:, 0:2], gcol, ALU.mult)
            stage(c)
            p_g, p_c, p_kk = pslot(c), pslot(c), pslot(c)
            K.MM(p_g, r.gbc[:], tri[:])
            K.MM(p_c[:, 0:2], tri[:], r.g2[:])
            K.MM(p_kk, w.kT[:, tsl], w.kT[:, tsl])
            if has_out:
                p_qk = pslot(c)
                K.MM(p_qk, w.kT[:, tsl], w.qT[:, tsl])
            yield
            K.act(r.Grow[:], p_g, AF.Copy)
            K.act(r.gcc[:], p_c[:, 0:2], AF.Copy)
            yield
            K.stt(r.t1[:], r.Grow[:], r.gcc[:, 0:1], ALU.subtract, mS[:], ALU.subtract)
            K.act(r.decS[:], r.t1[:], AF.Exp, scale=-1.0)
            if has_out:
                K.stt(r.t2[:], r.Grow[:], r.gcc[:, 0:1], ALU.subtract, mT[:], ALU.add)
                K.act(r.decT[:], r.t2[:], AF.Exp)
                K.act(r.EG[:], r.Grow[:], AF.Exp)
            K.act(r.sm[:, 0:1], r.gcc[:, 0:1], AF.Exp)
            K.ts("dve", r.sm[:, 1:2], r.gcc[:, 0:1], r.Grow[:, L:L + 1], ALU.subtract)
            K.act(r.sm[:, 2:3], r.sm[:, 1:2], AF.Exp, scale=-1.0)
            K.act(r.sm[:, 3:4], r.Grow[:, L:L + 1], AF.Exp)
            K.tt("dve", r.sm[:, 4:5], negb, r.sm[:, 0:1], ALU.mult)
            yield
            K.stt(P[0][:], p_kk, negb, ALU.mult, r.decS[:], ALU.mult)
            if has_out:
                K.tt("dve", r.attnT[:], p_qk, r.decT[:], ALU.mult)
                K.tt("dve", r.qeT[:], w.qT[:, tsl], r.EG[:], ALU.mult)
            K.ts("dve", r.R[:, 0:128], w.vTM[:, li, :], beta, ALU.mult)
            K.ts("dve", r.R[:, 128:256], w.kTM[:, li, :], r.sm[:, 4:5], ALU.mult)
            K.ts("dve", r.ke[:], w.kTM[:, li, :], r.sm[:, 2:3], ALU.mult)
            stage(c)
            p_q0 = pslot(c)
            K.TR(p_q0, P[0][:], Cn.ident[:])
            yield
            K.act(Q[0][:], p_q0, AF.Copy)
            K.tt("dve", Y[0][:], Q[0][:], Cn.ident[:], ALU.add)
            yield
            for j in range(1, 7):
                pp, qp = P[(j - 1) % 2], Q[(j - 1) % 2]
                stage(c)
                pa = pslot(c)
                K.MM(pa, qp[:], pp[:])
                if j < 6:
                    pb_ = pslot(c)
                    K.MM(pb_, pp[:], qp[:])
                yield
                K.act(P[j % 2][:], pa, AF.Copy)
                if j < 6:
                    K.act(Q[j % 2][:], pb_, AF.Copy)
                yield
                stage(c)
                pc = pslot(c)
                K.MM(pc, P[j % 2][:], Y[(j - 1) % 2][:])
                yield
                K.tt("dve", Y[j % 2][:], pc, Y[(j - 1) % 2][:], ALU.add)
                yield
            Yf = Y[0]
            stage(c)
            p_k = pslot(c)
            K.MM(p_k, r.R[:, 128:256], Yf[:])
            yield
            K.act(r.nk[:], p_k, AF.Copy)
            yield
            stage(c)
            p_v = pslot(c)
            K.MM(p_v, Yf[:], r.R[:, 0:128], start=True, stop=False)
            K.MM(p_v, r.nk[:], S_old[:], start=False, stop=True)
            yield
            K.act(r.vn[:], p_v, AF.Copy)
            yield
            stage(c)
            if has_out:
                p_o = pslot(c)
                K.MM(p_o, r.qeT[:], S_old[:], start=True, stop=False)
                K.MM(p_o, r.attnT[:], r.vn[:], start=False, stop=True)
            p_s = pslot(c)
            K.MM(p_s, r.ke[:], r.vn[:])
            yield
            K.stt(S_new[:], S_old[:], r.sm[:, 3:4], ALU.mult, p_s, ALU.add)
            if has_out:
                K.cp("dve", w.ost[:, li, :], p_o)
            yield

        def chain(c, d, h):
            r = res[c]
            K.memset("pool", r.S0[:], 0.0)
            if d == 0:
                wins = [(w0, True) for w0 in range(0, NT_OWN, 4)]
            else:
                wins = [(w0, w0 < NT_OWN) for w0 in range(NT_ALL - 4, -1, -4)]

            def load(i):
                w0, ho = wins[i]
                w = r.win[i % 2]
                tk = slice(w0 * 128, (w0 + 4) * 128)
                K.D("sp", w.kT[:], T.gk_d[h, :, tk])
                if ho:
                    K.D("sp", w.qT[:], T.gq_d[h, :, tk])
                K.D("sp", w.kTM[:], T.gkt_d[h].re("(n p) x -> p n x", p=128)[:, w0:w0 + 4, :])
                K.D("sp", w.vTM[:], T.gv_d[h].re("(n p) x -> p n x", p=128)[:, w0:w0 + 4, :])
            load(0)
            sidx = 0
            for i, (w0, ho) in enumerate(wins):
                if i + 1 < len(wins):
                    load(i + 1)
                w = r.win[i % 2]
                order = range(4) if d == 0 else range(3, -1, -1)
                for li in order:
                    yield from tile_steps(c, d, h, w0 + li, w, li, ho, sidx)
                    sidx += 1
                if ho:
                    K.D("sp", T.ob_d[d].re("(n p) x -> p n x", p=128)[:, w0:w0 + 4, h * 128:(h + 1) * 128], w.ost[:])
                yield

        allc = [(d, h) for d in (1, 0) for h in range(12)]
        for b0 in range(0, len(allc), CH):
            gens = [chain(c, d, h) for c, (d, h) in enumerate(allc[b0:b0 + CH])]
            while gens:
                for g in list(gens):
                    try:
                        next(g)
                    except StopIteration:
                        gens.remove(g)


PHASES[:] = [("pw1", phase_pw1), ("p0", phase0), ("p1", phase1), ("p2", phase2), ("p3", phase3), ("p4", phase4), ("p5", phase5)]


def phase6(K, T, Cn):
    with ExitStack() as es:
        gn = K.sb(es, "o_gn", [128, 128], F32)
        K.D("sp", gn[:], T.gnorm_rep[:, :])
        oa = [K.sb(es, f"o_a{i}", [128, 1536], F32) for i in range(2)]
        obb = [K.sb(es, f"o_b{i}", [128, 1536], F32) for i in range(2)]
        zz = [K.sb(es, f"o_z{i}", [128, 1536], BF16) for i in range(2)]
        sz = [K.sb(es, f"o_sz{i}", [128, 1536], F32) for i in range(2)]
        on = [K.sb(es, f"o_n{i}", [128, 1536], F32) for i in range(2)]
        o16 = [K.sb(es, f"o_16{i}", [128, 1536], BF16) for i in range(2)]
        ss = [K.sb(es, f"o_ss{i}", [128, 16], F32) for i in range(2)]
        junk = K.sb(es, "o_junk", [128, 128], F32)
        stg = [K.sb(es, f"o_st{i}", [128, 12, 512], BF16) for i in range(2)]
        pst = [K.ps(es, f"o_ps{i}", [128, 512], BF16) for i in range(6)]
        for t in range(NOWN // 128):
            b = t % 2
            tok = slice(t * 128, (t + 1) * 128)
            K.D("sp", oa[b][:], T.ob_d[0, tok, :])
            K.D("sp", obb[b][:], T.ob_d[1, tok, :])
            K.D("sp", zz[b][:], T.z_d[tok, :])
            K.tt("dve", oa[b][:], oa[b][:], obb[b][:], ALU.add)
            for h in range(12):
                K.act(junk[:], oa[b][:, h * 128:(h + 1) * 128], AF.Square, accum_out=ss[b][:, h:h + 1])
            K.act(ss[b][:, 0:12], ss[b][:, 0:12], AF.Sqrt, scale=1.0 / HD, bias=Cn.eps[:, 0:1])
            K.X("dve", "reciprocal", out=ss[b][:, 0:12], in_=ss[b][:, 0:12])
            for h in range(12):
                hs = slice(h * 128, (h + 1) * 128)
                K.stt(on[b][:, hs], oa[b][:, hs], ss[b][:, h:h + 1], ALU.mult, gn[:], ALU.mult)
            K.act(sz[b][:], zz[b][:], AF.Silu)
            K.tt("dve", o16[b][:], on[b][:], sz[b][:], ALU.mult)
            sg = stg[(t // 4) % 2]
            for g in range(3):
                pp = pst[(t % 2) * 3 + g]
                for q in range(4):
                    h = g * 4 + q
                    K.TR(pp[:, q * 128:(q + 1) * 128], o16[b][:, h * 128:(h + 1) * 128], Cn.ident_b[:])
                K.cp("act" if g % 2 else "dve", sg[:, 4 * g:4 * g + 4, (t % 4) * 128:(t % 4 + 1) * 128],
                     pp.re("p (s t) -> p s t", s=4))
            if t % 4 == 3:
                t0 = (t - 3) * 128
                K.D("sp", T.oT_d[4:16].re("c p t -> p c t")[:, :, t0:t0 + 512], sg[:])


def phase7(K, T, Cn):
    scale = 256.0 ** -0.5
    with ExitStack() as es:
        mT = K.sb(es, "m_T", [128, 16, 256], F32)
        K.D("sp", mT[:], T.memT.re("(c p) m -> p c m", p=128))
        sq = [K.sb(es, f"m_sq{i}", [128, 256], F32) for i in range(2)]
        rs = K.sb(es, "m_rs", [128, 256], F32)
        mn = K.sb(es, "m_n", [128, 16, 256], BF16)
        Wkv = K.sb(es, "m_W", [128, 16, 2048], BF16)
        wv = T.w_mem_kv_b.re("(c p) n -> p c n", p=128)
        K.D("sp", Wkv[:, 0:8, :], wv[:, 0:8, :])
        K.D("sp", Wkv[:, 8:16, :], wv[:, 8:16, :])
        mkT = K.sb(es, "m_kT", [128, 8, 256], BF16)
        mv = K.sb(es, "m_v", [128, 2, 1024], BF16)
        pss = [K.ps(es, f"m_pss{i}", [128, 256]) for i in range(2)]
        psb = pss
        pst = [K.ps(es, f"m_pst{i}", [128, 256], BF16) for i in range(2)]
        pso = [K.ps(es, f"m_pso{i}", [128, 256]) for i in range(2)]
        for c in range(16):
            K.act(sq[c % 2][:], mT[:, c, :], AF.Square)
            K.MM(psb[0][:, 0:256], Cn.ones[:], sq[c % 2][:], start=(c == 0), stop=(c == 15))
        K.act(rs[:], psb[0][:, 0:256], AF.Sqrt, scale=1.0 / D_MODEL, bias=Cn.eps[:, 0:1])
        K.X("dve", "reciprocal", out=rs[:], in_=rs[:])
        for c in range(16):
            K.stt(mn[:, c, :], mT[:, c, :], Cn.g_mem_c[:, c:c + 1], ALU.mult, rs[:], ALU.mult)
        for ct in range(8):
            p_ = psb[ct % 2]
            for c in range(16):
                K.MM(p_[:, 0:256], Wkv[:, c, ct * 128:(ct + 1) * 128], mn[:, c, :], start=(c == 0), stop=(c == 15))
            K.cp("act", mkT[:, ct, :], p_[:, 0:256])
        for mt in range(2):
            for cb in range(4):
                p_ = psb[(mt * 4 + cb) % 2]
                for c in range(16):
                    K.MM(p_[:], mn[:, c, mt * 128:(mt + 1) * 128], Wkv[:, c, 1024 + cb * 256:1024 + (cb + 1) * 256],
                         start=(c == 0), stop=(c == 15))
                K.cp("dve", mv[:, mt, cb * 256:(cb + 1) * 256], p_[:])
        mq = [K.sb(es, f"m_q{i}", [128, 8, 512], BF16) for i in range(2)]
        pb = [K.sb(es, f"m_p{i}", [128, 256], BF16) for i in range(3)]
        ptb = [K.sb(es, f"m_pt{i}", [128, 256], BF16) for i in range(3)]
        sml = [K.sb(es, f"m_sm{i}", [128, 8], F32) for i in range(3)]
        om = [K.sb(es, f"m_o{i}", [128, 1024], BF16) for i in range(2)]
        stg = [K.sb(es, f"m_st{i}", [128, 8, 512], BF16) for i in range(2)]
        pso2 = [K.ps(es, f"m_pt2{i}", [128, 512], BF16) for i in range(2)]
        k = 0
        for t in range(NOWN // 128):
            if t % 4 == 0:
                K.D("sp", mq[(t // 4) % 2][:], T.mq_d.re("c p t -> p c t")[:, :, t * 128:t * 128 + 512])
            mqb = mq[(t // 4) % 2]
            tl = slice((t % 4) * 128, (t % 4 + 1) * 128)
            for h in range(4):
                kk = k % 3
                p_s, p_t, p_o = pss[k % 2], pst[k % 2], pso[k % 2]
                k += 1
                K.MM(p_s[:], mqb[:, 2 * h, tl], mkT[:, 2 * h, :], start=True, stop=False)
                K.MM(p_s[:], mqb[:, 2 * h + 1, tl], mkT[:, 2 * h + 1, :], start=False, stop=True)
                K.X("dve", "reduce_max", out=sml[kk][:, 0:1], in_=p_s[:], axis=mybir.AxisListType.X)
                K.ts("dve", sml[kk][:, 1:2], sml[kk][:, 0:1], -scale, ALU.mult)
                K.act(pb[kk][:], p_s[:], AF.Exp, bias=sml[kk][:, 1:2], scale=scale, accum_out=sml[kk][:, 2:3])
                K.TR(p_t[:, 0:128], pb[kk][:, 0:128], Cn.ident_b[:])
                K.TR(p_t[:, 128:256], pb[kk][:, 128:256], Cn.ident_b[:])
                K.cp("act" if k % 2 else "dve", ptb[kk][:], p_t[:])
                K.MM(p_o[:], ptb[kk][:, 0:128], mv[:, 0, h * 256:(h + 1) * 256], start=True, stop=False)
                K.MM(p_o[:], ptb[kk][:, 128:256], mv[:, 1, h * 256:(h + 1) * 256], start=False, stop=True)
                K.X("dve", "reciprocal", out=sml[kk][:, 3:4], in_=sml[kk][:, 2:3])
                K.ts("dve", om[t % 2][:, h * 256:(h + 1) * 256], p_o[:], sml[kk][:, 3:4], ALU.mult)
            sg = stg[(t // 4) % 2]
            for g in range(2):
                pp = pso2[g]
                for q in range(4):
                    K.TR(pp[:, q * 128:(q + 1) * 128], om[t % 2][:, (g * 4 + q) * 128:(g * 4 + q + 1) * 128], Cn.ident_b[:])
                K.cp("act" if g else "dve", sg[:, 4 * g:4 * g + 4, tl], pp.re("p (s t) -> p s t", s=4))
            if t % 4 == 3:
                t0 = (t - 3) * 128
                K.D("sp", T.oT_d[16:24].re("c p t -> p c t")[:, :, t0:t0 + 512], sg[:])


def phase8(K, T, Cn):
    with ExitStack() as es:
        oT = K.sb(es, "e_oT", [128, 24, 512], BF16)
        aT = K.sb(es, "e_aT", [128, 16, 512], BF16)
        xT = K.sb(es, "e_xT", [128, 16, 512], F32)
        Wo = [K.sb(es, f"e_Wo{i}", [128, 24, 128], BF16) for i in range(2)]
        Wg = [K.sb(es, f"e_Wg{i}", [128, 16, 3, 128], BF16) for i in range(2)]
        Wu = [K.sb(es, f"e_Wu{i}", [128, 16, 128], BF16) for i in range(2)]
        mix = K.sb(es, "e_mix", [128, 16, 512], BF16)
        xn = K.sb(es, "e_xn", [128, 16, 512], BF16)
        gs = [K.sb(es, f"e_g{i}", [128, 512], F32) for i in range(3)]
        ta = K.sb(es, "e_ta", [128, 512], F32)
        tb = K.sb(es, "e_tb", [128, 512], F32)
        sq = [K.sb(es, f"e_sq{i}", [128, 512], F32) for i in range(2)]
        rs = K.sb(es, "e_rs", [128, 512], F32)
        Wr = K.sb(es, "e_Wr", [128, 16, 36], F32)
        K.D("sp", Wr[:], T.w_route.re("(c p) n -> p c n", p=128))
        brr = K.sb(es, "e_br", [128, 4], F32)
        K.D("sp", brr[:], T.b_route_rep[:, :])
        lg = [K.sb(es, f"e_lg{i}", [128, 36], F32) for i in range(2)]
        rw = [K.sb(es, f"e_rw{i}", [128, 64], F32) for i in range(2)]
        wf = [K.sb(es, f"e_wf{i}", [128, 4, 8], F32) for i in range(2)]
        bk = [K.ps(es, f"e_bk{i}", [128, 512]) for i in range(8)]
        nb = [0]

        def bank():
            nb[0] += 1
            return bk[nb[0] % 8]
        wov = T.w_o_cat_b.re("(c p) n -> p c n", p=128)
        wiv = T.w_in_b.re("(c p) n -> p c n", p=128)
        wuv = T.w_out_b.re("(c p) n -> p c n", p=128)
        branches = ((0, 4), (4, 16), (16, 24))
        for tg in range(NOWN // 512):
            tk = slice(tg * 512, (tg + 1) * 512)
            K.D("sp", oT[:, 0:12, :], T.oT_d.re("c p t -> p c t")[:, 0:12, tk])
            K.D("sp", oT[:, 12:24, :], T.oT_d.re("c p t -> p c t")[:, 12:24, tk])
            K.D("sp", aT[:], T.aT_d.re("(c p) t -> p c t", p=128)[:, :, tk])
            K.D("sp", xT[:], T.xT.re("(c p) t -> p c t", p=128)[:, :, tk])

            def loadw(j):
                K.D("sp", Wo[j % 2][:], wov[:, :, j * 128:(j + 1) * 128])
                for br in range(3):
                    c0 = C_GATE + br * 2048 + j * 128
                    K.D("sp", Wg[j % 2][:, :, br, :], wiv[:, :, c0:c0 + 128])
            loadw(0)
            for j in range(16):
                if j + 1 < 16:
                    loadw(j + 1)
                py = []
                for (c0, c1) in branches:
                    p_ = bank()
                    for c in range(c0, c1):
                        K.MM(p_[:], Wo[j % 2][:, c, :], oT[:, c, :], start=(c == c0), stop=(c == c1 - 1))
                    py.append(p_)
                for br in range(3):
                    p_ = bank()
                    for c in range(16):
                        K.MM(p_[:], Wg[j % 2][:, c, br, :], aT[:, c, :], start=(c == 0), stop=(c == 15))
                    K.act(gs[br][:], p_[:], AF.Sigmoid, bias=Cn.b_gate_c[:, br * 16 + j:br * 16 + j + 1])
                K.tt("dve", ta[:], py[0][:], gs[0][:], ALU.mult)
                K.tt("dve", tb[:], py[1][:], gs[1][:], ALU.mult)
                K.tt("pool", ta[:], ta[:], tb[:], ALU.add)
                K.tt("dve", tb[:], py[2][:], gs[2][:], ALU.mult)
                K.tt("pool", mix[:, j, :], ta[:], tb[:], ALU.add)
            K.D("sp", Wu[0][:], wuv[:, :, 0:128])
            for j in range(16):
                if j + 1 < 16:
                    K.D("sp", Wu[(j + 1) % 2][:], wuv[:, :, (j + 1) * 128:(j + 2) * 128])
                p_ = bank()
                for c in range(16):
                    K.MM(p_[:], Wu[j % 2][:, c, :], mix[:, c, :], start=(c == 0), stop=(c == 15))
                K.tt("dve", xT[:, j, :], xT[:, j, :], p_[:], ALU.add)
            K.D("sp", T.x1T_d.re("c p t -> p c t")[:, :, tk], xT[:])
            pss_ = bank()
            for c in range(16):
                K.act(sq[c % 2][:], xT[:, c, :], AF.Square)
                K.MM(pss_[:], Cn.ones[:], sq[c % 2][:], start=(c == 0), stop=(c == 15))
            K.act(rs[:], pss_[:], AF.Sqrt, scale=1.0 / D_MODEL, bias=Cn.eps[:, 0:1])
            K.X("dve", "reciprocal", out=rs[:], in_=rs[:])
            for c in range(16):
                K.ts("dve" if c % 2 else "pool", xT[:, c, :], xT[:, c, :], Cn.g_ffn_c[:, c:c + 1], ALU.mult)
                K.tt("dve", xn[:, c, :], xT[:, c, :], rs[:], ALU.mult)
            K.D("sp", T.xnT_d.re("c p t -> p c t")[:, :, tk], xn[:])
            for t4 in range(4):
                tl = slice(t4 * 128, (t4 + 1) * 128)
                l_, r_, w_ = lg[t4 % 2], rw[t4 % 2], wf[t4 % 2]
                pl, pt = bank(), bank()
                for c in range(16):
                    K.MM(pl[:, 0:36], xT[:, c, tl], Wr[:, c, :], start=(c == 0), stop=(c == 15))
                K.TR(pt[:, 0:128], rs[:, tl], Cn.ident[:])
                K.cp("dve", r_[:, 0:1], pt[:, 0:1])
                K.ts("dve", l_[:], pl[:, 0:36], r_[:, 0:1], ALU.mult)
                K.tt("dve", l_[:, 0:4], l_[:, 0:4], brr[:], ALU.add)
                K.X("dve", "reduce_max", out=r_[:, 1:2], in_=l_[:, 0:4], axis=mybir.AxisListType.X)
                K.ts("dve", r_[:, 4:8], l_[:, 0:4], r_[:, 1:2], ALU.is_equal)
                K.ts("dve", r_[:, 2:3], r_[:, 1:2], -1.0, ALU.mult)
                K.act(r_[:, 8:12], l_[:, 0:4], AF.Exp, bias=r_[:, 2:3], accum_out=r_[:, 3:4])
                K.X("dve", "reciprocal", out=r_[:, 3:4], in_=r_[:, 3:4])
                K.ts("dve", r_[:, 16:24], l_[:, 4:12], r_[:, 4:5], ALU.mult)
                for g in range(1, 4):
                    K.stt(r_[:, 16:24], l_[:, 4 + 8 * g:12 + 8 * g], r_[:, 4 + g:5 + g], ALU.mult, r_[:, 16:24], ALU.add)
                K.X("dve", "reduce_max", out=r_[:, 12:13], in_=r_[:, 16:24], axis=mybir.AxisListType.X)
                K.ts("dve", r_[:, 24:32], r_[:, 16:24], r_[:, 12:13], ALU.is_equal)
                K.stt(r_[:, 32:40], r_[:, 24:32], NEG, ALU.mult, r_[:, 16:24], ALU.add)
                K.X("dve", "reduce_max", out=r_[:, 13:14], in_=r_[:, 32:40], axis=mybir.AxisListType.X)
                K.ts("dve", r_[:, 40:48], r_[:, 32:40], r_[:, 13:14], ALU.is_equal)
                K.tt("dve", r_[:, 14:15], r_[:, 13:14], r_[:, 12:13], ALU.subtract)
                K.act(r_[:, 14:15], r_[:, 14:15], AF.Exp)
                K.ts("dve", r_[:, 15:16], r_[:, 14:15], 1.0, ALU.add)
                K.X("dve", "reciprocal", out=r_[:, 15:16], in_=r_[:, 15:16])
                K.tt("dve", r_[:, 48:49], r_[:, 15:16], r_[:, 3:4], ALU.mult)
                K.tt("dve", r_[:, 49:50], r_[:, 48:49], r_[:, 14:15], ALU.mult)
                K.ts("dve", r_[:, 56:64], r_[:, 24:32], r_[:, 48:49], ALU.mult)
                K.stt(r_[:, 56:64], r_[:, 40:48], r_[:, 49:50], ALU.mult, r_[:, 56:64], ALU.add)
                for g in range(4):
                    K.ts("dve", w_[:, g, :], r_[:, 56:64], r_[:, 4 + g:5 + g], ALU.mult)
                K.D("sp", T.wgt_d[tg * 512 + t4 * 128:tg * 512 + (t4 + 1) * 128, :], w_.re("p g e -> p (g e)"))


def phase9(K, T, Cn):
    with ExitStack() as es:
        xn = K.sb(es, "f_xn", [128, 16, 512], BF16)
        x1 = K.sb(es, "f_x1", [128, 16, 512], F32)
        acc = K.sb(es, "f_acc", [128, 4, 2048], F32)
        wgt = K.sb(es, "f_wgt", [128, 4, 32], F32)
        Wg = [K.sb(es, f"f_Wg{i}", [128, 16, 512], BF16) for i in range(2)]
        Wu = [K.sb(es, f"f_Wu{i}", [128, 16, 512], BF16) for i in range(2)]
        Wd = K.sb(es, "f_Wd", [128, 4, 2048], BF16)
        hm = [K.sb(es, f"f_hm{i}", [128, 4, 512], BF16) for i in range(2)]
        sg = [K.sb(es, f"f_sg{i}", [128, 512], F32) for i in range(2)]
        gf = K.sb(es, "f_gf", [128, D_MODEL], F32)
        K.D("sp", gf[:], T.g_final_rep[:, :])
        sm = K.sb(es, "f_sm", [128, 8], F32)
        junk = K.sb(es, "f_junk", [128, 2048], BF16)
        bk = [K.ps(es, f"f_bk{i}", [128, 512]) for i in range(8)]
        nb = [0]

        def bank():
            nb[0] += 1
            return bk[nb[0] % 8]
        gv = T.w_eg_b.re("(e c p) n -> e p c n", c=16, p=128)
        uv = T.w_eu_b.re("(e c p) n -> e p c n", c=16, p=128)
        dv = T.w_ed_b.re("(e c p) n -> e p c n", c=4, p=128)
        for tg in range(NOWN // 512):
            tk = slice(tg * 512, (tg + 1) * 512)
            K.D("sp", xn[:], T.xnT_d.re("c p t -> p c t")[:, :, tk])
            K.D("sp", x1[:], T.x1T_d.re("c p t -> p c t")[:, :, tk])
            K.D("sp", wgt[:], T.wgt_d.re("(n p) e -> p n e", p=128)[:, tg * 4:(tg + 1) * 4, :])
            for t4 in range(4):
                for cg in range(4):
                    p_ = bank()
                    for q in range(4):
                        K.TR(p_[:, q * 128:(q + 1) * 128], x1[:, cg * 4 + q, t4 * 128:(t4 + 1) * 128], Cn.ident[:])
                    K.cp("act" if cg % 2 else "dve", acc[:, t4, cg * 512:(cg + 1) * 512], p_[:])

            def loadw(e):
                K.D("sp", Wg[e % 2][:], gv[e])
                K.D("sp", Wu[e % 2][:], uv[e])
            loadw(0)
            for e in range(32):
                if e + 1 < 32:
                    loadw(e + 1)
                K.D("sp", Wd[:], dv[e])
                h_ = hm[e % 2]
                for f in range(4):
                    pg, pu = bank(), bank()
                    for c in range(16):
                        K.MM(pg[:], Wg[e % 2][:, c, f * 128:(f + 1) * 128], xn[:, c, :], start=(c == 0), stop=(c == 15))
                    for c in range(16):
                        K.MM(pu[:], Wu[e % 2][:, c, f * 128:(f + 1) * 128], xn[:, c, :], start=(c == 0), stop=(c == 15))
                    K.act(sg[f % 2][:], pg[:], AF.Silu)
                    K.tt("dve", h_[:, f, :], sg[f % 2][:], pu[:], ALU.mult)
                for t4 in range(4):
                    for cb in range(4):
                        pd = bank()
                        for f in range(4):
                            K.MM(pd[:], h_[:, f, t4 * 128:(t4 + 1) * 128], Wd[:, f, cb * 512:(cb + 1) * 512],
                                 start=(f == 0), stop=(f == 3))
                        cs = slice(cb * 512, (cb + 1) * 512)
                        K.stt(acc[:, t4, cs], pd[:], wgt[:, t4, e:e + 1], ALU.mult, acc[:, t4, cs], ALU.add)
            ost = x1.re("p c t -> p (c t)").re("p (n d) -> p n d", n=4)
            for t4 in range(4):
                K.act(junk[:], acc[:, t4, :], AF.Square, accum_out=sm[:, t4:t4 + 1])
            K.act(sm[:, 0:4], sm[:, 0:4], AF.Sqrt, scale=1.0 / D_MODEL, bias=Cn.eps[:, 0:1])
            K.X("dve", "reciprocal", out=sm[:, 0:4], in_=sm[:, 0:4])
            for t4 in range(4):
                K.stt(ost[:, t4, :], acc[:, t4, :], sm[:, t4:t4 + 1], ALU.mult, gf[:], ALU.mult)
            K.D("sp", T.out.re("(n p) d -> p n d", p=128)[:, tg * 4:(tg + 1) * 4, :], ost)


PHASES[:] = [("pw1", phase_pw1), ("pw2", phase_pw2), ("p0", phase0), ("p1", phase1), ("p2", phase2), ("p3", phase3),
             ("p4", phase4), ("p5", phase5), ("p6", phase6), ("p7", phase7), ("p8", phase8), ("p9", phase9)]
```
